# Optimizing a Trainium2 kernel written in Bass

```python
import jax, jax.numpy as jnp
from jax import lax
import numpy as np

D_MODEL = 1024
BATCH = 16
SEQ = 2048
DEPTH = 1

D_MIX = D_MODEL
HEAD_DIM = 64
ATTN_WIDTH = D_MIX // 2
N_ATTN_HEADS = ATTN_WIDTH // HEAD_DIM
CONV_WIDTH = D_MIX - ATTN_WIDTH
CONV_GROUP_DIM = 64
N_CONV_GROUPS = CONV_WIDTH // CONV_GROUP_DIM
CONV_K = 3
DILATED_PATTERNS = ((128, 1), (512, 4), (2048, 16))
ATTN_BLOCK = 128
IN_PROJ_WIDTH = 3 * ATTN_WIDTH + 3 * CONV_WIDTH
N_EXPERTS = 64
TOP_K = 8
N_GROUPS = 8
TOPK_GROUPS = 4
D_EXPERT = 256
D_SHARED = 256
ROUTED_SCALE = 2.5
MOE_TOKEN_BLOCK = 128
EPS = 1e-6
NEG_INF = -1e30

kernel_name = "hybrid_dilated_attn_shortconv_moe_adaln"


def rms_norm(x):
    xf = x.astype(jnp.float32)
    return (xf * lax.rsqrt(jnp.mean(xf * xf, axis=-1, keepdims=True) + EPS)).astype(x.dtype)


def modulate(h, shift, scale):
    return h * (1.0 + scale[:, None, :]) + shift[:, None, :]


def dilated_window_attention(q, k, v, window, dilation):
    b, t, h, dh = q.shape
    n_sub = window // dilation
    L = t // dilation
    nb = -(-L // ATTN_BLOCK)
    lp = nb * ATTN_BLOCK
    pad_end = lp - L

    def to_residue(a):
        return a.reshape(b, L, dilation, h, dh).transpose(0, 2, 1, 3, 4)

    qs, ks, vs = to_residue(q), to_residue(k), to_residue(v)
    qs = jnp.pad(qs, ((0, 0), (0, 0), (0, pad_end), (0, 0), (0, 0)))
    kv_pad = ((0, 0), (0, 0), (ATTN_BLOCK, pad_end), (0, 0), (0, 0))
    ks, vs = jnp.pad(ks, kv_pad), jnp.pad(vs, kv_pad)
    qb = qs.reshape(b, dilation, nb, ATTN_BLOCK, h, dh)

    def band(a):
        a = a.reshape(b, dilation, nb + 1, ATTN_BLOCK, h, dh)
        return jnp.concatenate([a[:, :, :-1], a[:, :, 1:]], axis=3)

    kb, vb = band(ks), band(vs)
    scale = 1.0 / np.sqrt(HEAD_DIM).astype(np.float32)
    s = jnp.einsum('bgnqhd,bgnkhd->bgnhqk', qb, kb) * scale
    qi = jnp.arange(ATTN_BLOCK)[:, None]
    ki = jnp.arange(2 * ATTN_BLOCK)[None, :]
    dist = qi + ATTN_BLOCK - ki
    key_pos = jnp.arange(nb)[:, None, None] * ATTN_BLOCK + ki[None] - ATTN_BLOCK
    valid = (dist >= 0)[None] & (dist <= n_sub)[None] & (key_pos >= 0)
    s = jnp.where(valid[:, None], s, NEG_INF)
    lse = jax.nn.logsumexp(s, axis=-1)
    p = jnp.exp(s - lse[..., None])
    o = jnp.einsum('bgnhqk,bgnkhd->bgnqhd', p, vb)
    o = o.reshape(b, dilation, lp, h, dh)[:, :, :L]
    o = o.transpose(0, 2, 1, 3, 4).reshape(b, t, h, dh)
    lse = lse.transpose(0, 1, 2, 4, 3).reshape(b, dilation, lp, h)[:, :, :L]
    lse = lse.transpose(0, 2, 1, 3).reshape(b, t, h)
    return o, lse


def mixture_of_dilations(q, k, v):
    outs, lses = [], []
    for window, dilation in DILATED_PATTERNS:
        o, l = dilated_window_attention(q, k, v, window, dilation)
        outs.append(o)
        lses.append(l)
    wts = jax.nn.softmax(jnp.stack(lses, axis=0), axis=0)
    return jnp.einsum('pbth,pbthd->bthd', wts, jnp.stack(outs, axis=0))


def causal_depthwise_conv(z, w):
    t = z.shape[1]
    zp = jnp.pad(z, ((0, 0), (CONV_K - 1, 0), (0, 0)))
    return sum(w[j] * zp[:, j:j + t] for j in range(CONV_K))


def group_rms(y, gain, group_dim):
    b, t, d = y.shape
    yg = rms_norm(y.reshape(b, t, d // group_dim, group_dim)).reshape(b, t, d)
    return yg * gain


def token_mixer(h, w_in, q_norm_g, k_norm_g, conv_w, attn_out_g, conv_out_g, w_out):
    b, t, _ = h.shape
    proj = h @ w_in
    a0, a1, a2 = ATTN_WIDTH, 2 * ATTN_WIDTH, 3 * ATTN_WIDTH
    q = proj[..., :a0].reshape(b, t, N_ATTN_HEADS, HEAD_DIM)
    k = proj[..., a0:a1].reshape(b, t, N_ATTN_HEADS, HEAD_DIM)
    v = proj[..., a1:a2].reshape(b, t, N_ATTN_HEADS, HEAD_DIM)
    gate_b = proj[..., a2:a2 + CONV_WIDTH]
    gate_c = proj[..., a2 + CONV_WIDTH:a2 + 2 * CONV_WIDTH]
    u = proj[..., a2 + 2 * CONV_WIDTH:]

    qf = (rms_norm(q) * q_norm_g).astype(jnp.float32)
    kf = (rms_norm(k) * k_norm_g).astype(jnp.float32)
    attn = mixture_of_dilations(qf, kf, v.astype(jnp.float32)).astype(h.dtype)
    attn = attn.reshape(b, t, ATTN_WIDTH)

    conv = gate_b * causal_depthwise_conv(gate_c * u, conv_w)

    y = jnp.concatenate([group_rms(attn, attn_out_g, HEAD_DIM),
                         group_rms(conv, conv_out_g, CONV_GROUP_DIM)], axis=-1)
    return y @ w_out


def moe_ffn(h, w_router, router_bias, w_e_gate, w_e_up, w_e_down, w_s_gate, w_s_up, w_s_down):
    b, t, d = h.shape
    n = b * t
    xt = h.reshape(n, d)
    scores = jax.nn.sigmoid(xt.astype(jnp.float32) @ w_router.astype(jnp.float32))
    biased = scores + router_bias.astype(jnp.float32)
    grp = biased.reshape(n, N_GROUPS, N_EXPERTS // N_GROUPS)
    grp_score = lax.top_k(grp, 2)[0].sum(-1)
    _, gidx = lax.top_k(grp_score, TOPK_GROUPS)
    gmask = jnp.max(jax.nn.one_hot(gidx, N_GROUPS, dtype=jnp.float32), axis=1) > 0
    emask = jnp.repeat(gmask, N_EXPERTS // N_GROUPS, axis=1)
    _, eidx = lax.top_k(jnp.where(emask, biased, NEG_INF), TOP_K)
    w_sel = jnp.take_along_axis(scores, eidx, axis=1)
    w_sel = w_sel / jnp.sum(w_sel, axis=-1, keepdims=True) * ROUTED_SCALE
    gates = jnp.sum(jax.nn.one_hot(eidx, N_EXPERTS, dtype=jnp.float32) * w_sel[..., None], axis=1)
    gates = gates.astype(h.dtype)

    def expert_block(args):
        xb, gb = args
        hg = jnp.einsum('nd,edf->nef', xb, w_e_gate)
        hu = jnp.einsum('nd,edf->nef', xb, w_e_up)
        hid = jax.nn.silu(hg) * hu * gb[..., None]
        return jnp.einsum('nef,efd->nd', hid, w_e_down)

    nblk = n // MOE_TOKEN_BLOCK
    routed = lax.map(expert_block, (xt.reshape(nblk, MOE_TOKEN_BLOCK, d),
                                    gates.reshape(nblk, MOE_TOKEN_BLOCK, N_EXPERTS)))
    shared = (jax.nn.silu(xt @ w_s_gate) * (xt @ w_s_up)) @ w_s_down
    return (routed.reshape(n, d) + shared).reshape(b, t, d)


def hybrid_layer(x, c, w_ada, b_ada, w_in, q_norm_g, k_norm_g, conv_w, attn_out_g, conv_out_g,
                 w_out, w_router, router_bias, w_e_gate, w_e_up, w_e_down, w_s_gate, w_s_up, w_s_down):
    mod = jax.nn.silu(c) @ w_ada + b_ada
    sh1, sc1, g1, sh2, sc2, g2 = jnp.split(mod, 6, axis=-1)
    h = modulate(rms_norm(x), sh1, sc1)
    x = x + g1[:, None, :] * token_mixer(h, w_in, q_norm_g, k_norm_g, conv_w,
                                         attn_out_g, conv_out_g, w_out)
    h = modulate(rms_norm(x), sh2, sc2)
    x = x + g2[:, None, :] * moe_ffn(h, w_router, router_bias, w_e_gate, w_e_up, w_e_down,
                                     w_s_gate, w_s_up, w_s_down)
    return x


def setup_inputs(seed: int = 0) -> dict:
    key = jax.random.key(seed)
    ks = jax.random.split(key, 20)
    f32 = jnp.float32
    L = DEPTH
    nrm = lambda k, shape, s: (jax.random.normal(k, shape, f32) * s).astype(f32)
    return {
        "x": nrm(ks[0], (BATCH, SEQ, D_MODEL), 1.0),
        "c": nrm(ks[1], (BATCH, D_MODEL), 1.0),
        "w_ada": nrm(ks[2], (L, D_MODEL, 6 * D_MODEL), 0.5 * D_MODEL ** -0.5),
        "b_ada": nrm(ks[3], (L, 6 * D_MODEL), 0.1),
        "w_in": nrm(ks[4], (L, D_MODEL, IN_PROJ_WIDTH), D_MODEL ** -0.5),
        "q_norm_g": 1.0 + nrm(ks[5], (L, HEAD_DIM), 0.02),
        "k_norm_g": 1.0 + nrm(ks[6], (L, HEAD_DIM), 0.02),
        "conv_w": nrm(ks[7], (L, CONV_K, CONV_WIDTH), CONV_K ** -0.5),
        "attn_out_g": 1.0 + nrm(ks[8], (L, ATTN_WIDTH), 0.02),
        "conv_out_g": 1.0 + nrm(ks[9], (L, CONV_WIDTH), 0.02),
        "w_out": nrm(ks[10], (L, D_MIX, D_MODEL), D_MIX ** -0.5),
        "w_router": nrm(ks[11], (L, D_MODEL, N_EXPERTS), D_MODEL ** -0.5),
        "router_bias": nrm(ks[12], (L, N_EXPERTS), 0.01),
        "w_e_gate": nrm(ks[13], (L, N_EXPERTS, D_MODEL, D_EXPERT), D_MODEL ** -0.5),
        "w_e_up": nrm(ks[14], (L, N_EXPERTS, D_MODEL, D_EXPERT), D_MODEL ** -0.5),
        "w_e_down": nrm(ks[15], (L, N_EXPERTS, D_EXPERT, D_MODEL), D_EXPERT ** -0.5),
        "w_s_gate": nrm(ks[16], (L, D_MODEL, D_SHARED), D_MODEL ** -0.5),
        "w_s_up": nrm(ks[17], (L, D_MODEL, D_SHARED), D_MODEL ** -0.5),
        "w_s_down": nrm(ks[18], (L, D_SHARED, D_MODEL), D_SHARED ** -0.5),
    }


def reference(x, c, w_ada, b_ada, w_in, q_norm_g, k_norm_g, conv_w, attn_out_g, conv_out_g,
              w_out, w_router, router_bias, w_e_gate, w_e_up, w_e_down, w_s_gate, w_s_up, w_s_down):
    for l in range(DEPTH):
        x = hybrid_layer(x, c, w_ada[l], b_ada[l], w_in[l], q_norm_g[l], k_norm_g[l], conv_w[l],
                         attn_out_g[l], conv_out_g[l], w_out[l], w_router[l], router_bias[l],
                         w_e_gate[l], w_e_up[l], w_e_down[l], w_s_gate[l], w_s_up[l], w_s_down[l])
    return x
```

```python
import numpy as np
from contextlib import ExitStack
import concourse.bass as bass
import concourse.mybir as mybir
from concourse.bass_utils import run_bass_kernel_spmd

F32 = mybir.dt.float32
BF16 = mybir.dt.bfloat16
ALU = mybir.AluOpType
AF = mybir.ActivationFunctionType
AX = mybir.AxisListType

ENGS = ("pe", "act", "dve", "pool", "sp")
NCORES = 8
T = 2048
D = 1024
NE = 65
EPS = 1e-6
BIG = 1.0e9


class _Op:
    __slots__ = ("eng", "fn", "is_dma", "semkey", "cum", "idx", "waits", "signal", "sigcount")


class Prog:
    def __init__(self, nc, same_engine_sync=True):
        self.nc = nc
        self.same_engine_sync = same_engine_sync
        self.by_eng = {e: [] for e in ENGS}
        self.last_w = {}
        self.readers = {}
        self.dma_count = {}
        self.waited_idx = {e: {} for e in ENGS}
        self.waited_dma = {e: {} for e in ENGS}
        self.out_dmas = []
        self.views = {}
        self.overl = {}
        self.front = {}

    def add_view(self, name, lo, hi):
        self.views[name] = (lo, hi)
        self.front[name] = {}
        self.overl[name] = []
        for o, (olo, ohi) in self.views.items():
            if o != name and olo < hi and lo < ohi:
                self.overl[name].append(o)
                self.overl[o].append(name)

    def add(self, eng, fn, reads=(), writes=(), dma=False, semkey=None, is_output=False):
        op = _Op()
        op.eng = eng
        op.fn = fn
        op.is_dma = dma
        op.signal = False
        op.idx = len(self.by_eng[eng])
        op.waits = []
        deps = []
        touched = None
        for r in reads:
            w = self.last_w.get(r)
            if w is not None:
                deps.append(w)
            n = r[0] if isinstance(r, tuple) else r
            if n in self.views:
                touched = (touched or set())
                touched.add(n)
        for w in writes:
            lw = self.last_w.get(w)
            if lw is not None:
                deps.append(lw)
            rd = self.readers.get(w)
            if rd:
                deps.extend(rd)
            n = w[0] if isinstance(w, tuple) else w
            if n in self.views:
                touched = (touched or set())
                touched.add(n)
        if touched:
            for n in touched:
                for o in self.overl[n]:
                    f = self.front[o]
                    if f:
                        deps.extend(f.values())
        if dma:
            op.semkey = semkey
            self.dma_count[semkey] = self.dma_count.get(semkey, 0) + 1
            op.cum = 16 * self.dma_count[semkey]
            if is_output:
                self.out_dmas.append(op)
        wi = self.waited_idx[eng]
        wd = self.waited_dma[eng]
        for d in deps:
            if d is op:
                continue
            if d.is_dma:
                cum = 16 * self.dma_count[d.semkey]
                if dma and semkey == d.semkey:
                    cum -= 16
                if wd.get(d.semkey, 0) >= cum:
                    continue
                wd[d.semkey] = cum
                op.waits.append(("d", d.semkey, cum))
            else:
                if d.eng == eng and (eng == "pe" or not self.same_engine_sync):
                    continue
                if wi.get(d.eng, -1) >= d.idx:
                    continue
                wi[d.eng] = d.idx
                d.signal = True
                op.waits.append(("e", d.eng, d))
        for r in reads:
            self.readers.setdefault(r, []).append(op)
        for w in writes:
            self.last_w[w] = op
            self.readers[w] = []
        if touched:
            fk = ("d", semkey) if dma else ("e", eng)
            for n in touched:
                self.front[n][fk] = op
        self.by_eng[eng].append(op)
        return op

    def emit(self, ctx):
        nc = self.nc
        esem = {e: ctx.enter_context(nc.semaphore("s_" + e)) for e in ENGS}
        dsem = {}
        for k in self.dma_count:
            dsem[k] = ctx.enter_context(nc.semaphore("d_%d" % len(dsem)))
        for e in ENGS:
            c = 0
            for op in self.by_eng[e]:
                if op.signal:
                    c += 1
                    op.sigcount = c
        block = ctx.enter_context(nc.Block())
        prog = self

        def run(e, engobj):
            for op in prog.by_eng[e]:
                need = {}
                for (kind, kk, vv) in op.waits:
                    key = (kind, kk)
                    v = vv if kind == "d" else vv.sigcount
                    if need.get(key, 0) < v:
                        need[key] = v
                for (kind, k), v in need.items():
                    engobj.wait_ge(dsem[k] if kind == "d" else esem[k], v)
                inst = op.fn(engobj)
                if op.is_dma:
                    inst.then_inc(dsem[op.semkey], 16)
                elif op.signal:
                    inst.then_inc(esem[e], 1)
            if e == "sp":
                fin = {}
                for op in prog.out_dmas:
                    fin[op.semkey] = max(fin.get(op.semkey, 0), op.cum)
                for k, v in fin.items():
                    engobj.wait_ge(dsem[k], v)

        @block.tensor
        def _(eng):
            run("pe", eng)

        @block.scalar
        def _(eng):
            run("act", eng)

        @block.vector
        def _(eng):
            run("dve", eng)

        @block.gpsimd
        def _(eng):
            run("pool", eng)

        @block.sync
        def _(eng):
            run("sp", eng)


SM_QG, SM_KG, SM_CW, SM_AG, SM_CG, SM_RB, SM_BA, SM_CT = 0, 1, 2, 14, 22, 26, 90, 138
SM_N = 154

X_WIN = 0
X_YTA = 0
X_A3T = 131072
X_QKV = 49152
X_WOA = 49152
X_WOC = 65536
X_H2F = 73728
X_YTC = 98304
X_XT = 114688
X_HTG = 131072
X_ACC = 0
X_WB = 65536
X_XT2 = 90112
X_MOET = 98304
X_WADA = 49152
X_BYTES = 139264


def build_program(n_seq=2, n_exp=NE, debug=False):
    nc = bass.Bass("TRN2", target_bir_lowering=False)
    dt = lambda name, shape, kind="ExternalInput", dtp=F32: nc.dram_tensor(name, shape, dtp, kind=kind).ap()
    x_d = dt("x", [2, T, D])
    small_d = dt("small", [128, SM_N])
    wr_d = dt("wr", [128, 8, 64])
    wada_d = dt("wada", [8, 128, 6144])
    bada_d = dt("bada", [1, 6144])
    win_d = dt("win", [128, 8, 3072])
    woa_d = dt("woa", [64, 8, 1024])
    woc_d = dt("woc", [128, 4, 1024])
    wg_d = dt("wg", [NE, 128, 8, 256])
    wu_d = dt("wu", [NE, 128, 8, 256])
    wd_d = dt("wd", [NE, 128, 2, 1024])
    out_d = dt("out", [2, T, D], kind="ExternalOutput")

    with ExitStack() as ctx:
        P = Prog(nc)
        ST = lambda name, shape, dtp: ctx.enter_context(nc.sbuf_tensor("sb_" + name, shape, dtp))
        X = ST("X", [128, X_BYTES // 2], BF16)

        def XV(name, off, nbytes, dtp, pattern=None, **kw):
            P.add_view(name, off, off + nbytes)
            v = X[:, off // 2:(off + nbytes) // 2]
            if dtp == F32:
                v = v.bitcast(F32)
            if pattern:
                v = v.rearrange(pattern, **kw)
            return v

        identF = ST("identF", [128, 128], F32)
        identB = ST("identB", [128, 128], BF16)
        onesF = ST("onesF", [128, 256], F32)
        mask2 = ST("mask2", [128, 256], BF16)
        blk1 = ST("blk1", [128, 128], BF16)
        wsel = ST("wsel", [128, 64], BF16)
        epsb = ST("epsb", [128, 1], F32)
        small = ST("small", [128, SM_N], F32)
        wr = ST("wr_sb", [128, 8, 64], F32)
        modT = ST("modT", [128, 32, 2], F32)
        scb = ST("scb", [128, 8, 2], F32)
        g1b = ST("g1b", [128, 1024], F32)
        g2b = ST("g2b", [128, 1024], F32)
        gates = ST("gates", [128, 16, NE], F32)
        h2T = ST("h2T", [128, 8, T], BF16)
        ssq = ST("ssq", [128, 4], F32)
        msq = ST("msq", [128, 4], F32)
        rst = ST("rst", [128, 4], F32)
        junk = ST("junk", [128, 1024], BF16)
        sqb = ST("sqb", [128, 512], BF16)
        msf = ST("msf", [128, 512], F32)
        rsf = ST("rsf", [128, 512], F32)
        Cs = ST("Cs", [128, 512], F32)
        zt = ST("zt", [128, 514], F32)
        zh = ST("zh", [128, 4, 2], F32)
        t1 = ST("t1", [128, 512], F32)
        cv = ST("cv", [128, 512], F32)
        cv2 = ST("cv2", [128, 512], F32)
        ps = [ctx.enter_context(nc.psum_tensor("ps%d" % i, [128, 512], F32)) for i in range(8)]

        wada = [XV("wada%d" % i, X_WADA + i * 24576, 24576, F32) for i in range(2)]
        badar = XV("badar", X_WADA + 49152, 24576, F32)
        screp = XV("screp", X_WADA + 73728, 4096, F32, "p (k n) -> p k n", k=8)
        Win = XV("Win", X_WIN, 49152, BF16, "p (k n) -> p k n", k=8)
        yTa = XV("yTa", X_YTA, 32768, BF16, "p (h t) -> p h t", h=8)
        qz1 = XV("qz1", X_YTA + 32768, 16384, BF16, "p (c t) -> p c t", c=4)
        qT = XV("qT", X_QKV, 16384, BF16, "p (c t) -> p c t", c=4)
        kT = XV("kT", X_QKV + 16384, 16384, BF16, "p (c t) -> p c t", c=4)
        vT = XV("vT", X_QKV + 32768, 16384, BF16, "p (c t) -> p c t", c=4)
        woa = XV("woa", X_WOA, 16384, BF16, "p (h n) -> p h n", h=8)
        woc = XV("woc", X_WOC, 8192, BF16, "p (c n) -> p c n", c=4)
        h2f = XV("h2f", X_H2F, 16384, F32, "p (k t) -> p k t", k=8)
        yTc = XV("yTc", X_YTC, 16384, BF16, "p (c t) -> p c t", c=4)
        xt = [XV("xt%d" % j, X_XT + j * 4096, 4096, F32) for j in range(4)]
        hTg = XV("hTg", X_HTG, 8192, BF16, "p (k t) -> p k t", k=8)
        Pb = [XV("Pb%d" % i, X_A3T + i * 512, 512, BF16) for i in range(2)]
        Pb += [ST("Pbx%d" % i, [128, 256], BF16) for i in range(3)]
        Vpat2 = ST("Vpat2", [128, 16, 65], BF16)
        Vpat = XV("Vpat0", X_A3T + 1024, 2080, BF16, "p (b n) -> p b n", b=16)
        sq65 = XV("sq65", X_A3T + 3200, 1024, BF16)
        tmpf = XV("tmpf", X_A3T + 4224, 2048, F32)
        rt = XV("rt", X_A3T + 6272, 1920, F32)
        acc = XV("acc", X_ACC, 65536, F32, "p (t n) -> p t n", t=16)
        wgb = [XV("wgb%d" % i, X_WB + i * 12288, 4096, BF16, "p (k n) -> p k n", k=8) for i in range(2)]
        wub = [XV("wub%d" % i, X_WB + i * 12288 + 4096, 4096, BF16, "p (k n) -> p k n", k=8) for i in range(2)]
        wdb = [XV("wdb%d" % i, X_WB + i * 12288 + 8192, 4096, BF16, "p (c n) -> p c n", c=2) for i in range(2)]
        xt2 = [XV("xt2_%d" % i, X_XT2 + i * 4096, 4096, F32) for i in range(2)]
        xt2 += [XV("xt2_%d" % (2 + i), X_MOET + 6144 + i * 4096, 4096, F32) for i in range(2)]
        sg = [XV("sg%d" % i, X_MOET + i * 1024, 1024, BF16) for i in range(2)]
        hid = [[XV("hid%d_%d" % (i, f), X_MOET + 2048 + (i * 2 + f) * 1024, 1024, BF16) for f in range(2)]
               for i in range(2)]

        def MM(out, lhsT, rhs, start, stop, reads, writes, sgc=False):
            if sgc:
                P.add("pe", lambda e: e.matmul(out, lhsT=lhsT, rhs=rhs, start=start, stop=stop,
                                               skip_group_check=True), reads, writes)
            else:
                P.add("pe", lambda e: e.matmul(out, lhsT=lhsT, rhs=rhs, start=start, stop=stop), reads, writes)

        def TR(out, in_, ident, reads, writes):
            P.add("pe", lambda e: e.transpose(out=out, in_=in_, identity=ident), reads, writes)

        def ACT(out, in_, func, reads, writes, bias=None, scale=None, accum_out=None):
            kw = {}
            if bias is not None:
                kw["bias"] = bias
            if scale is not None:
                kw["scale"] = scale
            if accum_out is not None:
                kw["accum_out"] = accum_out
            P.add("act", lambda e: e.activation(out=out, in_=in_, func=func, **kw), reads, writes)

        def TT(eng, out, in0, in1, op, reads, writes):
            P.add(eng, lambda e: e.tensor_tensor(out=out, in0=in0, in1=in1, op=op), reads, writes)

        def TS(eng, out, in0, s1, s2, op0, op1, reads, writes):
            if op1 is None:
                P.add(eng, lambda e: e.tensor_scalar(out=out, in0=in0, scalar1=s1, scalar2=None, op0=op0), reads, writes)
            else:
                P.add(eng, lambda e: e.tensor_scalar(out=out, in0=in0, scalar1=s1, scalar2=s2, op0=op0, op1=op1),
                      reads, writes)

        def STT(out, in0, scalar, in1, op0, op1, reads, writes):
            P.add("dve", lambda e: e.scalar_tensor_tensor(out=out, in0=in0, scalar=scalar, in1=in1, op0=op0, op1=op1),
                  reads, writes)

        def CP(eng, out, in_, reads, writes):
            P.add(eng, lambda e: e.tensor_copy(out=out, in_=in_), reads, writes)

        def MS(eng, ap, val, writes):
            P.add(eng, lambda e: e.memset(ap, val), (), writes)

        def DMA(eng, out, in_, reads, writes, semkey, is_output=False, **kw):
            P.add(eng, lambda e: e.dma_start(out=out, in_=in_, **kw), reads, writes, dma=True, semkey=semkey,
                  is_output=is_output)

        def PS(i):
            return ("ps", i)

        MS("pool", identF[:], 1.0, ["identF"])
        P.add("pool", lambda e: e.affine_select(out=identF[:], in_=identF[:], pattern=[[1, 128]],
                                               compare_op=ALU.is_equal, fill=0.0, base=0, channel_multiplier=-1),
              ["identF"], ["identF"])
        CP("dve", identB[:], identF[:], ["identF"], ["identB"])
        MS("pool", onesF[:], 1.0, ["onesF"])
        mtmp = tmpf
        MS("pool", mtmp[:, 0:256], 1.0, [("tmpf",)])
        P.add("pool", lambda e: e.affine_select(out=mtmp[:, 0:128], in_=mtmp[:, 0:128], pattern=[[1, 128]],
                                               compare_op=ALU.is_ge, fill=0.0, base=0, channel_multiplier=-1),
              [("tmpf",)], [("tmpf",)])
        P.add("pool", lambda e: e.affine_select(out=mtmp[:, 128:256], in_=mtmp[:, 128:256], pattern=[[-1, 128]],
                                               compare_op=ALU.is_ge, fill=0.0, base=0, channel_multiplier=1),
              [("tmpf",)], [("tmpf",)])
        TS("dve", mask2[:], mtmp[:, 0:256], -1.0, 30000.0, ALU.add, ALU.mult, [("tmpf",)], ["mask2"])
        MS("dve", blk1[:], 0.0, ["blk1"])
        MS("dve", blk1[0:64, 0:64], 1.0, ["blk1"])
        MS("dve", blk1[64:128, 64:128], 1.0, ["blk1"])
        MS("dve", wsel[:], 1.0 / 64.0, ["wsel"])
        MS("dve", wsel[64:128, :], EPS, ["wsel"])
        MS("dve", epsb[:], EPS, ["epsb"])
        MS("dve", gates[:, :, 64:65], 1.0, ["gates_sh"])
        DMA("sp", small[:], small_d, (), ["small"], "small")
        DMA("sp", wr[:], wr_d, (), ["wr"], "wr")
        TS("dve", small[:, SM_QG:SM_QG + 1], small[:, SM_QG:SM_QG + 1], 0.125, None, ALU.mult, None, ["small"], ["small"])
        ACT(scb[:].rearrange("p k b -> p (k b)"), small[:, SM_CT:SM_CT + 16], AF.Silu, ["small"], ["scb"])

        qg = small[:, SM_QG:SM_QG + 1]
        kg = small[:, SM_KG:SM_KG + 1]

        def rms_rstd(n):
            ACT(msq[:, 0:n], ssq[:, 0:n], AF.Ln, ["ssq", "epsb"], ["msq"], bias=epsb[:, 0:1], scale=1.0 / D)
            ACT(rst[:, 0:n], msq[:, 0:n], AF.Exp, ["msq"], ["rst"], scale=-0.5)

        for s in range(n_seq):
            b = s
            for k in range(8):
                DMA("pool", Win[:, k, :], win_d[:, k, :], (), [("Win", k)], "win", max_dma_last_dim=4096)
            for k in range(8):
                TS("dve", screp[:, k, :], onesF[:, 0:128], scb[:, k, b:b + 1], None, ALU.mult, None,
                   ["onesF", "scb"], [("screp", k)])
            DMA("sp", badar[0:1, :], bada_d, (), [("badar",)], "badar")
            mod_cols = [0, 1024, 3072, 4096]
            g_cols = [2048, 5120]
            for k in range(8):
                wa = wada[k % 2]
                wk = ("wada%d" % (k % 2),)
                if s == 0:
                    DMA("sp", wa[:, :], wada_d[k], (), [wk], "wada%d" % (k % 2))
                else:
                    DMA("sp", wa[:, 2048:3072], wada_d[k][:, 2048:3072], (), [wk], "wada%d" % (k % 2))
                    DMA("sp", wa[:, 5120:6144], wada_d[k][:, 5120:6144], (), [wk], "wada%d" % (k % 2))
                for part in range(4 if s == 0 else 0):
                    for fc in range(8):
                        j = part * 8 + fc
                        c0 = mod_cols[part] + fc * 128
                        MM(ps[0][:, 2 * j:2 * j + 2], wa[:, c0:c0 + 128], scb[:, k, :],
                           (k == 0 and j == 0), (k == 7), [wk, "scb"], [PS(0)], sgc=True)
                for gi in range(2):
                    for half in range(2):
                        c0 = g_cols[gi] + half * 512
                        MM(ps[1 + gi * 2 + half][:, :], screp[:, k, :], wa[:, c0:c0 + 512],
                           (k == 0), False, [wk, ("screp", k)], [PS(1 + gi * 2 + half)])
            for gi in range(2):
                for half in range(2):
                    c0 = g_cols[gi] + half * 512
                    MM(ps[1 + gi * 2 + half][:, :], onesF[0:1, 0:128], badar[0:1, c0:c0 + 512],
                       False, True, ["onesF", ("badar",)], [PS(1 + gi * 2 + half)])
                    dst = (g1b if gi == 0 else g2b)[:, half * 512:(half + 1) * 512]
                    CP("dve", dst, ps[1 + gi * 2 + half][:, :], (), [PS(1 + gi * 2 + half), ("gb", gi, half)])
            for part in range(4 if s == 0 else 0):
                bcol = SM_BA + mod_cols[part] // 128
                for bb in range(2):
                    TT("dve", modT[:, part * 8:(part + 1) * 8, bb],
                       ps[0][:, 16 * part + bb:16 * part + 16:2], small[:, bcol:bcol + 8], ALU.add,
                       ["small"], [PS(0), "modT"])
            for part in ((1, 3) if s == 0 else ()):
                TS("dve", modT[:, part * 8:(part + 1) * 8, :], modT[:, part * 8:(part + 1) * 8, :], 1.0, None,
                   ALU.add, None, ["modT"], ["modT"])
            sh1 = lambda k: modT[:, 0 + k, b:b + 1]
            sc1 = lambda k: modT[:, 8 + k, b:b + 1]
            sh2 = lambda k: modT[:, 16 + k, b:b + 1]
            sc2 = lambda k: modT[:, 24 + k, b:b + 1]


            def norm_and_transpose(g, sc, sh, dst_bf, dst_key, dst_f32=None, tbank=(0, 1)):
                for j in range(4):
                    ACT(junk[:], xt[j][:], AF.Square, [("xt%d" % j,)], ["junk", "junk2", "ssq"], accum_out=ssq[:, j:j + 1])
                rms_rstd(4)
                if dst_f32 is None:
                    for j in range(4):
                        xk = [("h2T", j, 0), ("h2T", j, 1)]
                        TS("dve", h2T[:, j, 0:1024], xt[j][:], rst[:, j:j + 1], None, ALU.mult, None,
                           [("xt%d" % j,), "rst"], xk)
                    for k in range(8):
                        bk = tbank[k % 2]
                        pb16 = ps[bk][:, 0:256].bitcast(BF16)
                        for j in range(4):
                            TR(pb16[:, j * 128:(j + 1) * 128], h2T[:, j, k * 128:(k + 1) * 128], identB[:],
                               [("h2T", j, 0), ("h2T", j, 1), "identB"], [PS(bk)])
                        ACT(dst_bf(k), pb16[:, :], AF.Identity, ["modT"], [PS(bk), dst_key(k)],
                            bias=sh(k), scale=sc(k))
                    return
                for j in range(4):
                    TS("dve", xt[j][:], xt[j][:], rst[:, j:j + 1], None, ALU.mult, None,
                       [("xt%d" % j,), "rst"], [("xt%d" % j,)])
                for k in range(8):
                    bk = tbank[k % 2]
                    for j in range(4):
                        TR(ps[bk][:, j * 128:(j + 1) * 128], xt[j][:, k * 128:(k + 1) * 128], identF[:],
                           [("xt%d" % j,), "identF"], [PS(bk)])
                    if dst_f32 is None:
                        ACT(dst_bf(k), ps[bk][:, :], AF.Identity, ["modT"], [PS(bk), dst_key(k)],
                            bias=sh(k), scale=sc(k))
                    else:
                        ACT(dst_f32[:, k, :], ps[bk][:, :], AF.Identity, ["modT"], [PS(bk), ("h2f", k)],
                            bias=sh(k), scale=sc(k))
                        CP("pool", dst_bf(k), dst_f32[:, k, :], [("h2f", k)], [dst_key(k)])

            def gn1(src_ap, src_keys_r, src_keys_w, par):
                sq_t, sq_k = [(sqb[:], "sqb"), (junk[:, 512:1024], "junk2")][par]
                ACT(sq_t, src_ap, AF.Square, src_keys_r, src_keys_w + [sq_k])
                MM(ps[par][:, :], blk1[:], sq_t, True, True, ["blk1", sq_k], [PS(par)])

            def gn2(src_ap, src_keys_r, src_keys_w, gain_ap, dst_ap, dst_key, par):
                m_t, m_k = [(msf[:], "msf"), (rsf[:], "rsf")][par]
                ACT(m_t, ps[par][:, :], AF.Ln, ["epsb"], [PS(par), m_k], bias=epsb[:, 0:1], scale=1.0 / 64.0)
                ACT(m_t, m_t, AF.Exp, [m_k], [m_k], scale=-0.5)
                STT(dst_ap, src_ap, gain_ap, m_t, ALU.mult, ALU.mult, src_keys_r + [m_k, "small"],
                    src_keys_w + [dst_key])

            pbrot = [0]
            for g in range(4):
                tok = slice(g * 512, (g + 1) * 512)
                for j in range(4):
                    ti = g * 4 + j
                    DMA("sp", xt[j][:], x_d[b, ti * 128:(ti + 1) * 128, :], (), [("xt%d" % j,)], "xt%d" % j)
                norm_and_transpose(g, sc1, sh1, lambda k: hTg[:, k, :], lambda k: ("hTg", k))

                def proj(fc, bank):
                    for k in range(8):
                        MM(ps[bank][:, :], Win[:, k, fc * 128:(fc + 1) * 128], hTg[:, k, :], k == 0, k == 7,
                           [("Win", k), ("hTg", k)], [PS(bank)])

                items = [("s", fc) for fc in range(12)] + [("c", cc) for cc in range(4)]
                ibanks = {}

                def nextbank():
                    bk = 2 + pbrot[0] % 6
                    pbrot[0] += 1
                    return bk

                def Pst(it):
                    kind, a = it
                    if kind == "s":
                        bk = nextbank()
                        proj(a, bk)
                        ibanks[it] = (bk,)
                    else:
                        bC, bU, bB = nextbank(), nextbank(), nextbank()
                        proj(16 + a, bC)
                        proj(20 + a, bU)
                        proj(12 + a, bB)
                        ibanks[it] = (bC, bU, bB)

                def E1(it, par):
                    kind, a = it
                    if kind == "s":
                        fc = a
                        (bank,) = ibanks[it]
                        c = fc % 4
                        if fc < 8:
                            gn1(ps[bank][:, :], [], [PS(bank)], par)
                        else:
                            ACT(vT[:, c, tok], ps[bank][:, :], AF.Copy, (), [PS(bank), ("vT", c, g)])
                    else:
                        cc = a
                        bC, bU, bB = ibanks[it]
                        cvt, cvk = [(cv, "cv"), (cv2, "cv2")][par]
                        ACT(Cs[:], ps[bC][:, :], AF.Copy, (), [PS(bC), "Cs"])
                        if g == 0:
                            MS("dve", zt[:, 0:2], 0.0, ["zt"])
                        else:
                            CP("dve", zt[:, 0:2], zh[:, cc, :], ["zh"], ["zt"])
                        TT("dve", zt[:, 2:514], ps[bU][:, :], Cs[:], ALU.mult, ["Cs"], [PS(bU), "zt"])
                        CP("dve", zh[:, cc, :], zt[:, 512:514], ["zt"], ["zh"])
                        cw = lambda jj: small[:, SM_CW + cc * 3 + jj:SM_CW + cc * 3 + jj + 1]
                        TS("dve", t1[:], zt[:, 0:512], cw(0), None, ALU.mult, None, ["zt", "small"], ["t1"])
                        STT(t1[:], zt[:, 1:513], cw(1), t1[:], ALU.mult, ALU.add, ["zt", "small"], ["t1"])
                        STT(t1[:], zt[:, 2:514], cw(2), t1[:], ALU.mult, ALU.add, ["zt", "small"], ["t1"])
                        TT("dve", cvt[:], t1[:], ps[bB][:, :], ALU.mult, ["t1"], [PS(bB), cvk])
                        gn1(cvt[:], [cvk], [], par)

                def E2(it, par):
                    kind, a = it
                    if kind == "s":
                        fc = a
                        (bank,) = ibanks[it]
                        c = fc % 4
                        if fc < 4:
                            gn2(ps[bank][:, :], [], [PS(bank)], qg, qT[:, c, tok], ("qT", c, g), par)
                        elif fc < 8:
                            gn2(ps[bank][:, :], [], [PS(bank)], kg, kT[:, c, tok], ("kT", c, g), par)
                    else:
                        cc = a
                        cvt, cvk = [(cv, "cv"), (cv2, "cv2")][par]
                        gn2(cvt[:], [cvk], [], small[:, SM_CG + cc:SM_CG + cc + 1], yTc[:, cc, tok],
                            ("yTc", cc, g), par)

                n_it = len(items)
                for i in range(n_it + 2):
                    if i < n_it:
                        Pst(items[i])
                    if 1 <= i <= n_it:
                        E1(items[i - 1], (i - 1) % 2)
                    if i >= 2:
                        E2(items[i - 2], (i - 2) % 2)

            Vp = [Vpat, Vpat2]
            for vb in range(2):
                MS("dve", Vp[vb][:, :, 64:65], 1.0, [("Vpat%d" % vb, "ones")])
            patterns = [(1, 16), (4, 4), (16, 1)]
            psV = ps[6][:, :].bitcast(BF16)
            SK = 4

            def tokslice(d, r, j, nblk=1):
                st = d * 128 * j + r
                return slice(st, st + d * (128 * nblk - 1) + 1, d)

            def pblocks(pi):
                d, nb = patterns[pi]
                return [(r, j) for r in range(d) for j in range(nb)]

            PJ = [(c, h, pi) for c in range(4) for h in range(2) for pi in range(3)]

            def emit_V(idx):
                c, h, pi = PJ[idx]
                d, nb = patterns[pi]
                vb = idx % 2
                blocks = pblocks(pi)
                for q4 in range(4):
                    for i4 in range(4):
                        r, j = blocks[q4 * 4 + i4]
                        TR(psV[:, i4 * 128:(i4 + 1) * 128], vT[:, c, tokslice(d, r, j)], identB[:],
                           [("vT", c, gg) for gg in range(4)] + ["identB"], [PS(6)])
                    src = psV[:, 0:512].rearrange("p (b f) -> p b f", b=4)[:, :, 64 * h:64 * h + 64]
                    CP("dve", Vp[vb][:, q4 * 4:q4 * 4 + 4, 0:64], src, (), [PS(6), ("Vpat%d" % vb, q4)])

            for cq in range(4):
                qkeys = [("qT", cq, gq) for gq in range(4)]
                MS("pool", qz1[0:64, cq, :], 0.0, [("qz1", cq)])
                CP("dve", qz1[64:128, cq, :], qT[64:128, cq, :], qkeys, [("qz1", cq)])
                MS("dve", qT[64:128, cq, :], 0.0, qkeys)
            qsrc = [qT, qz1]
            emit_V(0)
            gunit = 0
            for hidx in range(8):
                c, h = hidx // 2, hidx % 2
                head = hidx
                hp = slice(64 * h, 64 * h + 64)
                units = []
                first_of = {}
                for pi in range(3):
                    pj = hidx * 3 + pi
                    first_of[len(units)] = pj
                    for bi, (r, j) in enumerate(pblocks(pi)):
                        units.append((pj, pi, bi, r, j, gunit))
                        gunit += 1
                started = [False] * 4
                qk_reads = [("kT", c, gg) for gg in range(4)] + [("qT", c, gg) for gg in range(4)] + [("qz1", c)]

                def stageA(u):
                    pj, pi, bi, r, j, gu = u
                    d, nb = patterns[pi]
                    nq = 2 if j + 1 < nb else 1
                    sb = (4, 5, 7)[gu % 3]
                    pb = Pb[gu % 5]
                    pbk = ("Pb%d" % (gu % 5),)
                    MM(ps[sb][:, 0:nq * 128], kT[:, c, tokslice(d, r, j)], qsrc[h][:, c, tokslice(d, r, j, nq)],
                       True, False, qk_reads, [PS(sb)])
                    MM(ps[sb][:, 0:nq * 128], identB[:], mask2[:, 0:nq * 128], False, True,
                       ["identB", "mask2"], [PS(sb)])
                    ACT(pb[:, 0:nq * 128], ps[sb][:, 0:nq * 128], AF.Exp, (), [PS(sb), pbk])

                def stageB(u):
                    pj, pi, bi, r, j, gu = u
                    d, nb = patterns[pi]
                    nq = 2 if j + 1 < nb else 1
                    pb = Pb[gu % 5]
                    pbk = ("Pb%d" % (gu % 5),)
                    vb = pj % 2
                    for qi in range(nq):
                        jq = j + qi
                        if d == 1:
                            pieces = [(jq // 4, slice((jq % 4) * 128, (jq % 4) * 128 + 128), slice(qi * 128, qi * 128 + 128))]
                        elif d == 4:
                            pieces = [(jq, slice(r, 512, 4), slice(qi * 128, qi * 128 + 128))]
                        else:
                            pieces = [(bb, slice(r, 512, 16), slice(bb * 32, bb * 32 + 32)) for bb in range(4)]
                        for (bank, ocols, pcols) in pieces:
                            MM(ps[bank][0:65, ocols], Vp[vb][:, bi, 0:65], pb[:, pcols], not started[bank], False,
                               [pbk, ("Vpat%d" % vb, bi // 4), ("Vpat%d" % vb, "ones")], [PS(bank)], sgc=True)
                            started[bank] = True

                nu = len(units)
                for i in range(nu + SK):
                    if i < nu:
                        stageA(units[i])
                    if i >= SK:
                        stageB(units[i - SK])
                    f = i - (SK - 1)
                    if f in first_of and first_of[f] + 1 < len(PJ):
                        emit_V(first_of[f] + 1)
                sqs = [(sq65, ("sq65",)), (sqb, "sqb"), (junk[:, 0:512], "junk"), (junk[:, 512:1024], "junk2")]
                mss = [(msf, "msf"), (Cs, "Cs"), (t1, "t1"), (cv, "cv")]
                nbk = [6, 4, 5, 7]
                for bank in range(4):
                    ACT(sqs[bank][0][0:65, :], ps[bank][0:65, :], AF.Square, (), [PS(bank), sqs[bank][1]])
                for bank in range(4):
                    MM(ps[nbk[bank]][0:64, :], wsel[0:65, :], sqs[bank][0][0:65, :], True, True,
                       ["wsel", sqs[bank][1]], [PS(nbk[bank])])
                for bank in range(4):
                    ACT(mss[bank][0][0:64, :], ps[nbk[bank]][0:64, :], AF.Ln, (), [PS(nbk[bank]), mss[bank][1]])
                    ACT(mss[bank][0][0:64, :], mss[bank][0][0:64, :], AF.Exp, [mss[bank][1]], [mss[bank][1]], scale=-0.5)
                for bank in range(4):
                    tk = slice(bank * 512, (bank + 1) * 512)
                    mt, mk = mss[bank]
                    STT(yTa[0:64, head, tk], ps[bank][0:64, :], small[0:64, SM_AG + head:SM_AG + head + 1],
                        mt[0:64, :], ALU.mult, ALU.mult, [mk, "small"], [PS(bank), ("yTa", head, bank)])

            for hd in range(8):
                DMA("pool", woa[0:64, hd, :], woa_d[:, hd, :], (), [("woa", hd)], "woa", max_dma_last_dim=4096)
            for cc in range(4):
                DMA("pool", woc[:, cc, :], woc_d[:, cc, :], (), [("woc", cc)], "woc", max_dma_last_dim=4096)
            R = lambda a, n: rt[:, a:a + n]
            for g in range(4):
                for j in range(4):
                    ti = g * 4 + j
                    tk = slice(ti * 128, (ti + 1) * 128)
                    DMA("sp", xt[j][:], x_d[b, tk, :], (), [("xt%d" % j,)], "xt%d" % j)
                    for half in range(2):
                        bank = (j % 2) * 2 + half
                        cs = slice(half * 512, (half + 1) * 512)
                        for hd in range(8):
                            MM(ps[bank][:, :], yTa[0:64, hd, tk], woa[0:64, hd, cs], hd == 0, False,
                               [("yTa", hd, ti // 4), ("woa", hd)], [PS(bank)])
                        for cc in range(4):
                            MM(ps[bank][:, :], yTc[:, cc, tk], woc[:, cc, cs], False, cc == 3,
                               [("yTc", cc, g), ("woc", cc)], [PS(bank)])
                        TT("dve", tmpf[:], ps[bank][:, :], g1b[:, cs], ALU.mult, [("gb", 0, half)], [PS(bank), ("tmpf",)])
                        TT("dve", xt[j][:, cs], tmpf[:], xt[j][:, cs], ALU.add, [("tmpf",)], [("xt%d" % j,)])
                    DMA("sp", out_d[b, tk, :], xt[j][:], [("xt%d" % j,)], [("x1d", b, ti)], "x1st")
                norm_and_transpose(g, sc2, sh2, lambda k: h2T[:, k, g * 512:(g + 1) * 512], lambda k: ("h2T", k, g),
                                   dst_f32=h2f, tbank=(4, 5))
                for j in range(4):
                    ti = g * 4 + j
                    for k in range(8):
                        MM(ps[6][:, 0:64], h2f[:, k, j * 128:(j + 1) * 128], wr[:, k, :], k == 0, k == 7,
                           [("h2f", k), "wr"], [PS(6)])
                    en, sg_, bs, bs2, mb, sel = R(0, 64), R(64, 64), R(128, 64), R(192, 64), R(256, 64), R(320, 64)
                    m1, m2, gs, g8, gm, e8 = R(384, 8), R(392, 8), R(400, 8), R(408, 8), R(416, 8), R(424, 8)
                    ds_, rc = R(432, 1), R(433, 1)
                    RK = [("rt",)]
                    ACT(en, ps[6][:, 0:64], AF.Exp, (), [PS(6)] + RK, scale=-1.0)
                    TS("dve", en, en, 1.0, None, ALU.add, None, RK, RK)
                    P.add("dve", (lambda o, i: (lambda e: e.reciprocal(out=o, in_=i)))(sg_, en), RK, RK)
                    TT("dve", bs, sg_, small[:, SM_RB:SM_RB + 64], ALU.add, RK + ["small"], RK)
                    bs3 = bs.rearrange("p (a c) -> p a c", a=8)
                    bs23 = bs2.rearrange("p (a c) -> p a c", a=8)
                    mb3 = mb.rearrange("p (a c) -> p a c", a=8)
                    P.add("dve", (lambda o, i: (lambda e: e.tensor_reduce(out=o, in_=i, axis=AX.X, op=ALU.max)))(m1, bs3), RK, RK)
                    TT("dve", bs23, bs3, m1.unsqueeze(2).to_broadcast([128, 8, 8]), ALU.is_equal, RK, RK)
                    STT(bs2, bs2, -BIG, bs, ALU.mult, ALU.add, RK, RK)
                    P.add("dve", (lambda o, i: (lambda e: e.tensor_reduce(out=o, in_=i, axis=AX.X, op=ALU.max)))(m2, bs23), RK, RK)
                    TT("dve", gs, m1, m2, ALU.add, RK, RK)
                    P.add("dve", (lambda o, i: (lambda e: e.max(out=o, in_=i)))(g8, gs), RK, RK)
                    TS("dve", gm, gs, g8[:, 3:4], None, ALU.is_ge, None, RK, RK)
                    TS("dve", gm, gm, -1.0, BIG, ALU.add, ALU.mult, RK, RK)
                    TT("dve", mb3, bs3, gm.unsqueeze(2).to_broadcast([128, 8, 8]), ALU.add, RK, RK)
                    P.add("dve", (lambda o, i: (lambda e: e.max(out=o, in_=i)))(e8, mb), RK, RK)
                    TS("dve", sel, mb, e8[:, 7:8], None, ALU.is_ge, None, RK, RK)
                    TT("dve", sel, sel, sg_, ALU.mult, RK, RK)
                    P.add("dve", (lambda o, i: (lambda e: e.tensor_reduce(out=o, in_=i, axis=AX.X, op=ALU.add)))(ds_, sel), RK, RK)
                    P.add("dve", (lambda o, i: (lambda e: e.reciprocal(out=o, in_=i)))(rc, ds_), RK, RK)
                    TS("dve", gates[:, ti, 0:64], sel, rc, 2.5, ALU.mult, ALU.mult, RK, [("gates", ti)])

            def load_expert(e):
                i = e % 2
                DMA("pool", wgb[i][:], wg_d[e], (), [("wgb%d" % i,)], "wg%d" % i, max_dma_last_dim=4096)
                DMA("pool", wub[i][:], wu_d[e], (), [("wub%d" % i,)], "wu%d" % i, max_dma_last_dim=4096)
                DMA("pool", wdb[i][:], wd_d[e], (), [("wdb%d" % i,)], "wd%d" % i, max_dma_last_dim=4096)

            load_expert(0)
            drot = [0]

            def GU(e, g, fc, par):
                i = e % 2
                tok = slice(g * 512, (g + 1) * 512)
                fs = slice(fc * 128, (fc + 1) * 128)
                bA, bB = fc, 2 + fc
                for k in range(8):
                    MM(ps[bA][:, :], wgb[i][:, k, fs], h2T[:, k, tok], k == 0, k == 7,
                       [("wgb%d" % i,), ("h2T", k, g)], [PS(bA)])
                for k in range(8):
                    MM(ps[bB][:, :], wub[i][:, k, fs], h2T[:, k, tok], k == 0, k == 7,
                       [("wub%d" % i,), ("h2T", k, g)], [PS(bB)])
                ACT(sg[fc][:], ps[bA][:, :], AF.Silu, (), [PS(bA), ("sg%d" % fc,)])
                TT("dve", hid[par][fc][:], ps[bB][:, :], sg[fc][:], ALU.mult, [("sg%d" % fc,)],
                   [PS(bB), ("hid%d_%d" % (par, fc),)])

            def DOWN(e, g, par):
                i = e % 2
                for j in range(4):
                    ti = g * 4 + j
                    for half in range(2):
                        bank = 4 + drot[0] % 4
                        drot[0] += 1
                        cs = slice(half * 512, (half + 1) * 512)
                        for fc in range(2):
                            MM(ps[bank][:, :], hid[par][fc][:, j * 128:(j + 1) * 128], wdb[i][:, fc, cs],
                               fc == 0, fc == 1, [("hid%d_%d" % (par, fc),), ("wdb%d" % i,)], [PS(bank)])
                        gk = [("gates", ti)] if e < 64 else ["gates_sh"]
                        if e == 0:
                            TS("dve", acc[:, ti, cs], ps[bank][:, :], gates[:, ti, e:e + 1], None, ALU.mult, None,
                               gk, [PS(bank), ("acc", ti, half)])
                        else:
                            STT(acc[:, ti, cs], ps[bank][:, :], gates[:, ti, e:e + 1], acc[:, ti, cs],
                                ALU.mult, ALU.add, gk, [PS(bank), ("acc", ti, half)])

            def load_x1(ti):
                tk_ = slice(ti * 128, (ti + 1) * 128)
                DMA("sp", xt2[ti % 4][:], out_d[b, tk_, :], [("x1d", b, ti)], [("xt2_%d" % (ti % 4),)],
                    "x1ld%d" % (ti % 4))

            munits = [(e, g) for e in range(n_exp) for g in range(4)]
            for idx, (e, g) in enumerate(munits):
                if idx == len(munits) - 4:
                    for ti0 in range(4):
                        load_x1(ti0)
                GU(e, g, 0, idx % 2)
                if idx > 0:
                    pe_, pg_ = munits[idx - 1]
                    DOWN(pe_, pg_, (idx - 1) % 2)
                if g == 0 and e + 1 < n_exp:
                    load_expert(e + 1)
                GU(e, g, 1, idx % 2)
            pe_, pg_ = munits[-1]
            DOWN(pe_, pg_, (len(munits) - 1) % 2)

            for ti in range(16):
                tk = slice(ti * 128, (ti + 1) * 128)
                xb_ = xt2[ti % 4]
                xk = ("xt2_%d" % (ti % 4),)
                for half in range(2):
                    cs = slice(half * 512, (half + 1) * 512)
                    TT("dve", acc[:, ti, cs], acc[:, ti, cs], g2b[:, cs], ALU.mult, [("gb", 1, half)], [("acc", ti, half)])
                    TT("pool" if half else "dve", acc[:, ti, cs], acc[:, ti, cs], xb_[:, cs], ALU.add, [xk],
                       [("acc", ti, half)])
                DMA("sp", out_d[b, tk, :], acc[:, ti, :], [("acc", ti, 0), ("acc", ti, 1)], [("x1d", b, ti)],
                    "outst", is_output=True)
                if ti + 4 < 16:
                    load_x1(ti + 4)

        P.emit(ctx)
    return nc


_PROG = None


def _layouts(inp):
    f = lambda a: np.ascontiguousarray(np.asarray(a, dtype=np.float32))
    L = {}
    L["wr"] = f(inp["w_router"][0].reshape(8, 128, 64).transpose(1, 0, 2))
    L["wada"] = f(inp["w_ada"][0].reshape(8, 128, 6144))
    L["bada"] = f(inp["b_ada"][0].reshape(1, 6144))
    L["win"] = f(inp["w_in"][0].reshape(8, 128, 3072).transpose(1, 0, 2))
    wo = inp["w_out"][0]
    L["woa"] = f(wo[:512].reshape(8, 64, 1024).transpose(1, 0, 2))
    L["woc"] = f(wo[512:].reshape(4, 128, 1024).transpose(1, 0, 2))
    wg = np.concatenate([inp["w_e_gate"][0], inp["w_s_gate"][0][None]], 0)
    wu = np.concatenate([inp["w_e_up"][0], inp["w_s_up"][0][None]], 0)
    wd = np.concatenate([inp["w_e_down"][0], inp["w_s_down"][0][None]], 0)
    L["wg"] = f(wg.reshape(NE, 8, 128, 256).transpose(0, 2, 1, 3))
    L["wu"] = f(wu.reshape(NE, 8, 128, 256).transpose(0, 2, 1, 3))
    L["wd"] = f(wd.reshape(NE, 2, 128, 1024).transpose(0, 2, 1, 3))
    sm = np.zeros((128, SM_N), np.float32)
    sm[:, SM_QG] = np.tile(inp["q_norm_g"][0], 2)
    sm[:, SM_KG] = np.tile(inp["k_norm_g"][0], 2)
    sm[:, SM_CW:SM_CW + 12] = inp["conv_w"][0].reshape(3, 4, 128).transpose(2, 1, 0).reshape(128, 12)
    sm[:64, SM_AG:SM_AG + 8] = inp["attn_out_g"][0].reshape(8, 64).T
    sm[:, SM_CG:SM_CG + 4] = inp["conv_out_g"][0].reshape(4, 128).T
    sm[:, SM_RB:SM_RB + 64] = np.broadcast_to(inp["router_bias"][0][None, :], (128, 64))
    sm[:, SM_BA:SM_BA + 48] = inp["b_ada"][0].reshape(48, 128).T
    L["small"] = sm
    return L


def kernel(**inputs):
    global _PROG
    inp = {k: np.asarray(v) for k, v in inputs.items()}
    L = _layouts(inp)
    x = np.ascontiguousarray(inp["x"], dtype=np.float32)
    c = np.asarray(inp["c"], dtype=np.float32)
    if _PROG is None:
        _PROG = build_program()
    in_maps = []
    for core in range(NCORES):
        m = dict(L)
        sm = L["small"].copy()
        cc = c[2 * core:2 * core + 2]
        sm[:, SM_CT:SM_CT + 16] = cc.reshape(2, 8, 128).transpose(2, 1, 0).reshape(128, 16)
        m["small"] = sm
        m["x"] = x[2 * core:2 * core + 2]
        in_maps.append(m)
    res = run_bass_kernel_spmd(_PROG, in_maps, core_ids=list(range(NCORES)))
    out = np.concatenate([r["out"] for r in res.results], axis=0)
    return out.astype(np.float32, copy=False)
```

```python
import numpy as np
from contextlib import ExitStack
import concourse.bass as bass
import concourse.mybir as mybir
from concourse.bass_utils import run_bass_kernel_spmd

F32 = mybir.dt.float32
BF16 = mybir.dt.bfloat16
ALU = mybir.AluOpType
AF = mybir.ActivationFunctionType
AX = mybir.AxisListType

ENGS = ("pe", "act", "dve", "pool", "sp")
NCORES = 8
T = 2048
D = 1024
NE = 65
EPS = 1e-6
BIG = 1.0e9


class _Op:
    __slots__ = ("eng", "fn", "is_dma", "semkey", "cum", "idx", "waits", "signal", "sigcount")


class Prog:
    def __init__(self, nc, same_engine_sync=True):
        self.nc = nc
        self.same_engine_sync = same_engine_sync
        self.by_eng = {e: [] for e in ENGS}
        self.last_w = {}
        self.readers = {}
        self.dma_count = {}
        self.waited_idx = {e: {} for e in ENGS}
        self.waited_dma = {e: {} for e in ENGS}
        self.out_dmas = []
        self.views = {}
        self.overl = {}
        self.front = {}

    def add_view(self, name, lo, hi):
        self.views[name] = (lo, hi)
        self.front[name] = {}
        self.overl[name] = []
        for o, (olo, ohi) in self.views.items():
            if o != name and olo < hi and lo < ohi:
                self.overl[name].append(o)
                self.overl[o].append(name)

    def add(self, eng, fn, reads=(), writes=(), dma=False, semkey=None, is_output=False):
        op = _Op()
        op.eng = eng
        op.fn = fn
        op.is_dma = dma
        op.signal = False
        op.idx = len(self.by_eng[eng])
        op.waits = []
        deps = []
        touched = None
        for r in reads:
            w = self.last_w.get(r)
            if w is not None:
                deps.append(w)
            n = r[0] if isinstance(r, tuple) else r
            if n in self.views:
                touched = (touched or set())
                touched.add(n)
        for w in writes:
            lw = self.last_w.get(w)
            if lw is not None:
                deps.append(lw)
            rd = self.readers.get(w)
            if rd:
                deps.extend(rd)
            n = w[0] if isinstance(w, tuple) else w
            if n in self.views:
                touched = (touched or set())
                touched.add(n)
        if touched:
            for n in touched:
                for o in self.overl[n]:
                    f = self.front[o]
                    if f:
                        deps.extend(f.values())
        if dma:
            op.semkey = semkey
            self.dma_count[semkey] = self.dma_count.get(semkey, 0) + 1
            op.cum = 16 * self.dma_count[semkey]
            if is_output:
                self.out_dmas.append(op)
        wi = self.waited_idx[eng]
        wd = self.waited_dma[eng]
        for d in deps:
            if d is op:
                continue
            if d.is_dma:
                cum = 16 * self.dma_count[d.semkey]
                if dma and semkey == d.semkey:
                    cum -= 16
                if wd.get(d.semkey, 0) >= cum:
                    continue
                wd[d.semkey] = cum
                op.waits.append(("d", d.semkey, cum))
            else:
                if d.eng == eng and (eng == "pe" or not self.same_engine_sync):
                    continue
                if wi.get(d.eng, -1) >= d.idx:
                    continue
                wi[d.eng] = d.idx
                d.signal = True
                op.waits.append(("e", d.eng, d))
        for r in reads:
            self.readers.setdefault(r, []).append(op)
        for w in writes:
            self.last_w[w] = op
            self.readers[w] = []
        if touched:
            fk = ("d", semkey) if dma else ("e", eng)
            for n in touched:
                self.front[n][fk] = op
        self.by_eng[eng].append(op)
        return op

    def emit(self, ctx):
        nc = self.nc
        esem = {e: ctx.enter_context(nc.semaphore("s_" + e)) for e in ENGS}
        dsem = {}
        for k in self.dma_count:
            dsem[k] = ctx.enter_context(nc.semaphore("d_%d" % len(dsem)))
        for e in ENGS:
            c = 0
            for op in self.by_eng[e]:
                if op.signal:
                    c += 1
                    op.sigcount = c
        block = ctx.enter_context(nc.Block())
        prog = self

        def run(e, engobj):
            for op in prog.by_eng[e]:
                need = {}
                for (kind, kk, vv) in op.waits:
                    key = (kind, kk)
                    v = vv if kind == "d" else vv.sigcount
                    if need.get(key, 0) < v:
                        need[key] = v
                for (kind, k), v in need.items():
                    engobj.wait_ge(dsem[k] if kind == "d" else esem[k], v)
                inst = op.fn(engobj)
                if op.is_dma:
                    inst.then_inc(dsem[op.semkey], 16)
                elif op.signal:
                    inst.then_inc(esem[e], 1)
            if e == "sp":
                fin = {}
                for op in prog.out_dmas:
                    fin[op.semkey] = max(fin.get(op.semkey, 0), op.cum)
                for k, v in fin.items():
                    engobj.wait_ge(dsem[k], v)

        @block.tensor
        def _(eng):
            run("pe", eng)

        @block.scalar
        def _(eng):
            run("act", eng)

        @block.vector
        def _(eng):
            run("dve", eng)

        @block.gpsimd
        def _(eng):
            run("pool", eng)

        @block.sync
        def _(eng):
            run("sp", eng)


SM_QG, SM_KG, SM_CW, SM_AG, SM_CG, SM_RB, SM_BA, SM_CT = 0, 1, 2, 14, 22, 26, 90, 138
SM_N = 154

X_WIN = 0
X_YTA = 0
X_A3T = 131072
X_QKV = 49152
X_WOA = 49152
X_WOC = 65536
X_H2F = 73728
X_YTC = 98304
X_XT = 114688
X_HTG = 131072
X_ACC = 0
X_WB = 65536
X_XT2 = 90112
X_MOET = 98304
X_WADA = 49152
X_BYTES = 139264


def build_program(n_seq=2, n_exp=NE, debug=False):
    nc = bass.Bass("TRN2", target_bir_lowering=False)
    dt = lambda name, shape, kind="ExternalInput", dtp=F32: nc.dram_tensor(name, shape, dtp, kind=kind).ap()
    x_d = dt("x", [2, T, D])
    small_d = dt("small", [128, SM_N])
    wr_d = dt("wr", [128, 8, 64])
    wada_d = dt("wada", [8, 128, 6144])
    bada_d = dt("bada", [1, 6144])
    win_d = dt("win", [128, 8, 3072])
    woa_d = dt("woa", [64, 8, 1024])
    woc_d = dt("woc", [128, 4, 1024])
    wg_d = dt("wg", [NE, 128, 8, 256])
    wu_d = dt("wu", [NE, 128, 8, 256])
    wd_d = dt("wd", [NE, 128, 2, 1024])
    out_d = dt("out", [2, T, D], kind="ExternalOutput")

    with ExitStack() as ctx:
        P = Prog(nc)
        ST = lambda name, shape, dtp: ctx.enter_context(nc.sbuf_tensor("sb_" + name, shape, dtp))
        X = ST("X", [128, X_BYTES // 2], BF16)

        def XV(name, off, nbytes, dtp, pattern=None, **kw):
            P.add_view(name, off, off + nbytes)
            v = X[:, off // 2:(off + nbytes) // 2]
            if dtp == F32:
                v = v.bitcast(F32)
            if pattern:
                v = v.rearrange(pattern, **kw)
            return v

        identF = ST("identF", [128, 128], F32)
        identB = ST("identB", [128, 128], BF16)
        onesF = ST("onesF", [128, 256], F32)
        mask2 = ST("mask2", [128, 256], BF16)
        blk1 = ST("blk1", [128, 128], BF16)
        wsel = ST("wsel", [128, 64], BF16)
        epsb = ST("epsb", [128, 1], F32)
        small = ST("small", [128, SM_N], F32)
        wr = ST("wr_sb", [128, 8, 64], F32)
        modT = ST("modT", [128, 32, 2], F32)
        scb = ST("scb", [128, 8, 2], F32)
        g1b = ST("g1b", [128, 1024], F32)
        g2b = ST("g2b", [128, 1024], F32)
        gates = ST("gates", [128, 16, NE], F32)
        h2T = ST("h2T", [128, 8, T], BF16)
        ssq = ST("ssq", [128, 4], F32)
        msq = ST("msq", [128, 4], F32)
        rst = ST("rst", [128, 4], F32)
        junk = ST("junk", [128, 1024], BF16)
        sqb = ST("sqb", [128, 512], BF16)
        msf = ST("msf", [128, 512], F32)
        rsf = ST("rsf", [128, 512], F32)
        Cs = ST("Cs", [128, 512], F32)
        zt = ST("zt", [128, 514], F32)
        zh = ST("zh", [128, 4, 2], F32)
        t1 = ST("t1", [128, 512], F32)
        cv = ST("cv", [128, 512], F32)
        cv2 = ST("cv2", [128, 512], F32)
        ps = [ctx.enter_context(nc.psum_tensor("ps%d" % i, [128, 512], F32)) for i in range(8)]

        wada = [XV("wada%d" % i, X_WADA + i * 24576, 24576, F32) for i in range(2)]
        badar = XV("badar", X_WADA + 49152, 24576, F32)
        screp = XV("screp", X_WADA + 73728, 4096, F32, "p (k n) -> p k n", k=8)
        Win = XV("Win", X_WIN, 49152, BF16, "p (k n) -> p k n", k=8)
        yTa = XV("yTa", X_YTA, 32768, BF16, "p (h t) -> p h t", h=8)
        qz1 = XV("qz1", X_YTA + 32768, 16384, BF16, "p (c t) -> p c t", c=4)
        qT = XV("qT", X_QKV, 16384, BF16, "p (c t) -> p c t", c=4)
        kT = XV("kT", X_QKV + 16384, 16384, BF16, "p (c t) -> p c t", c=4)
        vT = XV("vT", X_QKV + 32768, 16384, BF16, "p (c t) -> p c t", c=4)
        woa = XV("woa", X_WOA, 16384, BF16, "p (h n) -> p h n", h=8)
        woc = XV("woc", X_WOC, 8192, BF16, "p (c n) -> p c n", c=4)
        h2f = XV("h2f", X_H2F, 16384, F32, "p (k t) -> p k t", k=8)
        yTc = XV("yTc", X_YTC, 16384, BF16, "p (c t) -> p c t", c=4)
        xt = [XV("xt%d" % j, X_XT + j * 4096, 4096, F32) for j in range(4)]
        hTg = XV("hTg", X_HTG, 8192, BF16, "p (k t) -> p k t", k=8)
        Pb = [XV("Pb%d" % i, X_A3T + i * 512, 512, BF16) for i in range(2)]
        Pb += [ST("Pbx%d" % i, [128, 256], BF16) for i in range(2)]
        Vpat2 = ST("Vpat2", [128, 16, 65], BF16)
        Vpat = XV("Vpat0", X_A3T + 1024, 2080, BF16, "p (b n) -> p b n", b=16)
        sq65 = XV("sq65", X_A3T + 3200, 1024, BF16)
        tmpf = XV("tmpf", X_A3T + 4224, 2048, F32)
        rt = XV("rt", X_A3T + 6272, 1920, F32)
        acc = XV("acc", X_ACC, 65536, F32, "p (t n) -> p t n", t=16)
        wgb = [XV("wgb%d" % i, X_WB + i * 12288, 4096, BF16, "p (k n) -> p k n", k=8) for i in range(2)]
        wub = [XV("wub%d" % i, X_WB + i * 12288 + 4096, 4096, BF16, "p (k n) -> p k n", k=8) for i in range(2)]
        wdb = [XV("wdb%d" % i, X_WB + i * 12288 + 8192, 4096, BF16, "p (c n) -> p c n", c=2) for i in range(2)]
        xt2 = [XV("xt2_%d" % i, X_XT2 + i * 4096, 4096, F32) for i in range(2)]
        xt2 += [XV("xt2_%d" % (2 + i), X_MOET + 6144 + i * 4096, 4096, F32) for i in range(2)]
        sg = [XV("sg%d" % i, X_MOET + i * 1024, 1024, BF16) for i in range(2)]
        hid = [[XV("hid%d_%d" % (i, f), X_MOET + 2048 + (i * 2 + f) * 1024, 1024, BF16) for f in range(2)]
               for i in range(2)]

        def MM(out, lhsT, rhs, start, stop, reads, writes, sgc=False):
            if sgc:
                P.add("pe", lambda e: e.matmul(out, lhsT=lhsT, rhs=rhs, start=start, stop=stop,
                                               skip_group_check=True), reads, writes)
            else:
                P.add("pe", lambda e: e.matmul(out, lhsT=lhsT, rhs=rhs, start=start, stop=stop), reads, writes)

        def TR(out, in_, ident, reads, writes):
            P.add("pe", lambda e: e.transpose(out=out, in_=in_, identity=ident), reads, writes)

        def ACT(out, in_, func, reads, writes, bias=None, scale=None, accum_out=None):
            kw = {}
            if bias is not None:
                kw["bias"] = bias
            if scale is not None:
                kw["scale"] = scale
            if accum_out is not None:
                kw["accum_out"] = accum_out
            P.add("act", lambda e: e.activation(out=out, in_=in_, func=func, **kw), reads, writes)

        def TT(eng, out, in0, in1, op, reads, writes):
            P.add(eng, lambda e: e.tensor_tensor(out=out, in0=in0, in1=in1, op=op), reads, writes)

        def TS(eng, out, in0, s1, s2, op0, op1, reads, writes):
            if op1 is None:
                P.add(eng, lambda e: e.tensor_scalar(out=out, in0=in0, scalar1=s1, scalar2=None, op0=op0), reads, writes)
            else:
                P.add(eng, lambda e: e.tensor_scalar(out=out, in0=in0, scalar1=s1, scalar2=s2, op0=op0, op1=op1),
                      reads, writes)

        def STT(out, in0, scalar, in1, op0, op1, reads, writes):
            P.add("dve", lambda e: e.scalar_tensor_tensor(out=out, in0=in0, scalar=scalar, in1=in1, op0=op0, op1=op1),
                  reads, writes)

        def CP(eng, out, in_, reads, writes):
            P.add(eng, lambda e: e.tensor_copy(out=out, in_=in_), reads, writes)

        def MS(eng, ap, val, writes):
            P.add(eng, lambda e: e.memset(ap, val), (), writes)

        def DMA(eng, out, in_, reads, writes, semkey, is_output=False, **kw):
            P.add(eng, lambda e: e.dma_start(out=out, in_=in_, **kw), reads, writes, dma=True, semkey=semkey,
                  is_output=is_output)

        def PS(i):
            return ("ps", i)

        MS("pool", identF[:], 1.0, ["identF"])
        P.add("pool", lambda e: e.affine_select(out=identF[:], in_=identF[:], pattern=[[1, 128]],
                                               compare_op=ALU.is_equal, fill=0.0, base=0, channel_multiplier=-1),
              ["identF"], ["identF"])
        CP("dve", identB[:], identF[:], ["identF"], ["identB"])
        MS("pool", onesF[:], 1.0, ["onesF"])
        mtmp = tmpf
        MS("pool", mtmp[:, 0:256], 1.0, [("tmpf",)])
        P.add("pool", lambda e: e.affine_select(out=mtmp[:, 0:128], in_=mtmp[:, 0:128], pattern=[[1, 128]],
                                               compare_op=ALU.is_ge, fill=0.0, base=0, channel_multiplier=-1),
              [("tmpf",)], [("tmpf",)])
        P.add("pool", lambda e: e.affine_select(out=mtmp[:, 128:256], in_=mtmp[:, 128:256], pattern=[[-1, 128]],
                                               compare_op=ALU.is_ge, fill=0.0, base=0, channel_multiplier=1),
              [("tmpf",)], [("tmpf",)])
        TS("dve", mask2[:], mtmp[:, 0:256], -1.0, 30000.0, ALU.add, ALU.mult, [("tmpf",)], ["mask2"])
        MS("dve", blk1[:], 0.0, ["blk1"])
        MS("dve", blk1[0:64, 0:64], 1.0, ["blk1"])
        MS("dve", blk1[64:128, 64:128], 1.0, ["blk1"])
        MS("dve", wsel[:], 1.0 / 64.0, ["wsel"])
        MS("dve", wsel[64:128, :], EPS, ["wsel"])
        MS("dve", epsb[:], EPS, ["epsb"])
        MS("dve", gates[:, :, 64:65], 1.0, ["gates_sh"])
        DMA("sp", small[:], small_d, (), ["small"], "small")
        DMA("sp", wr[:], wr_d, (), ["wr"], "wr")
        TS("dve", small[:, SM_QG:SM_QG + 1], small[:, SM_QG:SM_QG + 1], 0.125, None, ALU.mult, None, ["small"], ["small"])
        ACT(scb[:].rearrange("p k b -> p (k b)"), small[:, SM_CT:SM_CT + 16], AF.Silu, ["small"], ["scb"])

        qg = small[:, SM_QG:SM_QG + 1]
        kg = small[:, SM_KG:SM_KG + 1]

        def rms_rstd(n):
            ACT(msq[:, 0:n], ssq[:, 0:n], AF.Ln, ["ssq", "epsb"], ["msq"], bias=epsb[:, 0:1], scale=1.0 / D)
            ACT(rst[:, 0:n], msq[:, 0:n], AF.Exp, ["msq"], ["rst"], scale=-0.5)

        for s in range(n_seq):
            b = s
            for k in range(8):
                DMA("pool", Win[:, k, :], win_d[:, k, :], (), [("Win", k)], "win", max_dma_last_dim=4096)
            for k in range(8):
                TS("dve", screp[:, k, :], onesF[:, 0:128], scb[:, k, b:b + 1], None, ALU.mult, None,
                   ["onesF", "scb"], [("screp", k)])
            DMA("sp", badar[0:1, :], bada_d, (), [("badar",)], "badar")
            mod_cols = [0, 1024, 3072, 4096]
            g_cols = [2048, 5120]
            for k in range(8):
                wa = wada[k % 2]
                wk = ("wada%d" % (k % 2),)
                if s == 0:
                    DMA("sp", wa[:, :], wada_d[k], (), [wk], "wada%d" % (k % 2))
                else:
                    DMA("sp", wa[:, 2048:3072], wada_d[k][:, 2048:3072], (), [wk], "wada%d" % (k % 2))
                    DMA("sp", wa[:, 5120:6144], wada_d[k][:, 5120:6144], (), [wk], "wada%d" % (k % 2))
                for part in range(4 if s == 0 else 0):
                    for fc in range(8):
                        j = part * 8 + fc
                        c0 = mod_cols[part] + fc * 128
                        MM(ps[0][:, 2 * j:2 * j + 2], wa[:, c0:c0 + 128], scb[:, k, :],
                           (k == 0 and j == 0), (k == 7), [wk, "scb"], [PS(0)], sgc=True)
                for gi in range(2):
                    for half in range(2):
                        c0 = g_cols[gi] + half * 512
                        MM(ps[1 + gi * 2 + half][:, :], screp[:, k, :], wa[:, c0:c0 + 512],
                           (k == 0), False, [wk, ("screp", k)], [PS(1 + gi * 2 + half)])
            for gi in range(2):
                for half in range(2):
                    c0 = g_cols[gi] + half * 512
                    MM(ps[1 + gi * 2 + half][:, :], onesF[0:1, 0:128], badar[0:1, c0:c0 + 512],
                       False, True, ["onesF", ("badar",)], [PS(1 + gi * 2 + half)])
                    dst = (g1b if gi == 0 else g2b)[:, half * 512:(half + 1) * 512]
                    CP("dve", dst, ps[1 + gi * 2 + half][:, :], (), [PS(1 + gi * 2 + half), ("gb", gi, half)])
            for part in range(4 if s == 0 else 0):
                bcol = SM_BA + mod_cols[part] // 128
                for bb in range(2):
                    TT("dve", modT[:, part * 8:(part + 1) * 8, bb],
                       ps[0][:, 16 * part + bb:16 * part + 16:2], small[:, bcol:bcol + 8], ALU.add,
                       ["small"], [PS(0), "modT"])
            for part in ((1, 3) if s == 0 else ()):
                TS("dve", modT[:, part * 8:(part + 1) * 8, :], modT[:, part * 8:(part + 1) * 8, :], 1.0, None,
                   ALU.add, None, ["modT"], ["modT"])
            sh1 = lambda k: modT[:, 0 + k, b:b + 1]
            sc1 = lambda k: modT[:, 8 + k, b:b + 1]
            sh2 = lambda k: modT[:, 16 + k, b:b + 1]
            sc2 = lambda k: modT[:, 24 + k, b:b + 1]


            def norm_and_transpose(g, sc, sh, dst_bf, dst_key, dst_f32=None, tbank=(0, 1)):
                for j in range(4):
                    ACT(junk[:], xt[j][:], AF.Square, [("xt%d" % j,)], ["junk", "junk2", "ssq"], accum_out=ssq[:, j:j + 1])
                rms_rstd(4)
                if dst_f32 is None:
                    for j in range(4):
                        xk = [("h2T", j, 0), ("h2T", j, 1)]
                        TS("dve", h2T[:, j, 0:1024], xt[j][:], rst[:, j:j + 1], None, ALU.mult, None,
                           [("xt%d" % j,), "rst"], xk)
                    for k in range(8):
                        bk = tbank[k % 2]
                        pb16 = ps[bk][:, 0:256].bitcast(BF16)
                        for j in range(4):
                            TR(pb16[:, j * 128:(j + 1) * 128], h2T[:, j, k * 128:(k + 1) * 128], identB[:],
                               [("h2T", j, 0), ("h2T", j, 1), "identB"], [PS(bk)])
                        ACT(dst_bf(k), pb16[:, :], AF.Identity, ["modT"], [PS(bk), dst_key(k)],
                            bias=sh(k), scale=sc(k))
                    return
                for j in range(4):
                    TS("dve", xt[j][:], xt[j][:], rst[:, j:j + 1], None, ALU.mult, None,
                       [("xt%d" % j,), "rst"], [("xt%d" % j,)])
                for k in range(8):
                    bk = tbank[k % 2]
                    for j in range(4):
                        TR(ps[bk][:, j * 128:(j + 1) * 128], xt[j][:, k * 128:(k + 1) * 128], identF[:],
                           [("xt%d" % j,), "identF"], [PS(bk)])
                    if dst_f32 is None:
                        ACT(dst_bf(k), ps[bk][:, :], AF.Identity, ["modT"], [PS(bk), dst_key(k)],
                            bias=sh(k), scale=sc(k))
                    else:
                        ACT(dst_f32[:, k, :], ps[bk][:, :], AF.Identity, ["modT"], [PS(bk), ("h2f", k)],
                            bias=sh(k), scale=sc(k))
                        CP("pool", dst_bf(k), dst_f32[:, k, :], [("h2f", k)], [dst_key(k)])

            def gn1(src_ap, src_keys_r, src_keys_w, par):
                sq_t, sq_k = [(sqb[:], "sqb"), (junk[:, 512:1024], "junk2")][par]
                ACT(sq_t, src_ap, AF.Square, src_keys_r, src_keys_w + [sq_k])
                MM(ps[par][:, :], blk1[:], sq_t, True, True, ["blk1", sq_k], [PS(par)])

            def gn2(src_ap, src_keys_r, src_keys_w, gain_ap, dst_ap, dst_key, par):
                m_t, m_k = [(msf[:], "msf"), (rsf[:], "rsf")][par]
                ACT(m_t, ps[par][:, :], AF.Ln, ["epsb"], [PS(par), m_k], bias=epsb[:, 0:1], scale=1.0 / 64.0)
                ACT(m_t, m_t, AF.Exp, [m_k], [m_k], scale=-0.5)
                STT(dst_ap, src_ap, gain_ap, m_t, ALU.mult, ALU.mult, src_keys_r + [m_k, "small"],
                    src_keys_w + [dst_key])

            pbrot = [0]
            for g in range(4):
                tok = slice(g * 512, (g + 1) * 512)
                for j in range(4):
                    ti = g * 4 + j
                    DMA("sp", xt[j][:], x_d[b, ti * 128:(ti + 1) * 128, :], (), [("xt%d" % j,)], "xt%d" % j)
                norm_and_transpose(g, sc1, sh1, lambda k: hTg[:, k, :], lambda k: ("hTg", k))

                def proj(fc, bank):
                    for k in range(8):
                        MM(ps[bank][:, :], Win[:, k, fc * 128:(fc + 1) * 128], hTg[:, k, :], k == 0, k == 7,
                           [("Win", k), ("hTg", k)], [PS(bank)])

                items = [("s", fc) for fc in range(12)] + [("c", cc) for cc in range(4)]
                ibanks = {}

                def nextbank():
                    bk = 2 + pbrot[0] % 6
                    pbrot[0] += 1
                    return bk

                def Pst(it):
                    kind, a = it
                    if kind == "s":
                        bk = nextbank()
                        proj(a, bk)
                        ibanks[it] = (bk,)
                    else:
                        bC, bU, bB = nextbank(), nextbank(), nextbank()
                        proj(16 + a, bC)
                        proj(20 + a, bU)
                        proj(12 + a, bB)
                        ibanks[it] = (bC, bU, bB)

                def E1(it, par):
                    kind, a = it
                    if kind == "s":
                        fc = a
                        (bank,) = ibanks[it]
                        c = fc % 4
                        if fc < 8:
                            gn1(ps[bank][:, :], [], [PS(bank)], par)
                        else:
                            ACT(vT[:, c, tok], ps[bank][:, :], AF.Copy, (), [PS(bank), ("vT", c, g)])
                    else:
                        cc = a
                        bC, bU, bB = ibanks[it]
                        cvt, cvk = [(cv, "cv"), (cv2, "cv2")][par]
                        ACT(Cs[:], ps[bC][:, :], AF.Copy, (), [PS(bC), "Cs"])
                        if g == 0:
                            MS("dve", zt[:, 0:2], 0.0, ["zt"])
                        else:
                            CP("dve", zt[:, 0:2], zh[:, cc, :], ["zh"], ["zt"])
                        TT("dve", zt[:, 2:514], ps[bU][:, :], Cs[:], ALU.mult, ["Cs"], [PS(bU), "zt"])
                        CP("dve", zh[:, cc, :], zt[:, 512:514], ["zt"], ["zh"])
                        cw = lambda jj: small[:, SM_CW + cc * 3 + jj:SM_CW + cc * 3 + jj + 1]
                        TS("dve", t1[:], zt[:, 0:512], cw(0), None, ALU.mult, None, ["zt", "small"], ["t1"])
                        STT(t1[:], zt[:, 1:513], cw(1), t1[:], ALU.mult, ALU.add, ["zt", "small"], ["t1"])
                        STT(t1[:], zt[:, 2:514], cw(2), t1[:], ALU.mult, ALU.add, ["zt", "small"], ["t1"])
                        TT("dve", cvt[:], t1[:], ps[bB][:, :], ALU.mult, ["t1"], [PS(bB), cvk])
                        gn1(cvt[:], [cvk], [], par)

                def E2(it, par):
                    kind, a = it
                    if kind == "s":
                        fc = a
                        (bank,) = ibanks[it]
                        c = fc % 4
                        if fc < 4:
                            gn2(ps[bank][:, :], [], [PS(bank)], qg, qT[:, c, tok], ("qT", c, g), par)
                        elif fc < 8:
                            gn2(ps[bank][:, :], [], [PS(bank)], kg, kT[:, c, tok], ("kT", c, g), par)
                    else:
                        cc = a
                        cvt, cvk = [(cv, "cv"), (cv2, "cv2")][par]
                        gn2(cvt[:], [cvk], [], small[:, SM_CG + cc:SM_CG + cc + 1], yTc[:, cc, tok],
                            ("yTc", cc, g), par)

                n_it = len(items)
                for i in range(n_it + 2):
                    if i < n_it:
                        Pst(items[i])
                    if 1 <= i <= n_it:
                        E1(items[i - 1], (i - 1) % 2)
                    if i >= 2:
                        E2(items[i - 2], (i - 2) % 2)

            Vp = [Vpat, Vpat2]
            for vb in range(2):
                MS("dve", Vp[vb][:, :, 64:65], 1.0, [("Vpat%d" % vb, "ones")])
            patterns = [(1, 16), (4, 4), (16, 1)]
            psV = ps[6][:, :].bitcast(BF16)
            SK = 3

            def tokslice(d, r, j, nblk=1):
                st = d * 128 * j + r
                return slice(st, st + d * (128 * nblk - 1) + 1, d)

            def pblocks(pi):
                d, nb = patterns[pi]
                return [(r, j) for r in range(d) for j in range(nb)]

            PJ = [(c, h, pi) for c in range(4) for h in range(2) for pi in range(3)]

            def emit_V(idx):
                c, h, pi = PJ[idx]
                d, nb = patterns[pi]
                vb = idx % 2
                blocks = pblocks(pi)
                for q4 in range(4):
                    for i4 in range(4):
                        r, j = blocks[q4 * 4 + i4]
                        TR(psV[:, i4 * 128:(i4 + 1) * 128], vT[:, c, tokslice(d, r, j)], identB[:],
                           [("vT", c, gg) for gg in range(4)] + ["identB"], [PS(6)])
                    src = psV[:, 0:512].rearrange("p (b f) -> p b f", b=4)[:, :, 64 * h:64 * h + 64]
                    CP("dve", Vp[vb][:, q4 * 4:q4 * 4 + 4, 0:64], src, (), [PS(6), ("Vpat%d" % vb, q4)])

            for cq in range(4):
                qkeys = [("qT", cq, gq) for gq in range(4)]
                MS("pool", qz1[0:64, cq, :], 0.0, [("qz1", cq)])
                CP("dve", qz1[64:128, cq, :], qT[64:128, cq, :], qkeys, [("qz1", cq)])
                MS("dve", qT[64:128, cq, :], 0.0, qkeys)
            qsrc = [qT, qz1]
            emit_V(0)
            gunit = 0
            for hidx in range(8):
                c, h = hidx // 2, hidx % 2
                head = hidx
                hp = slice(64 * h, 64 * h + 64)
                units = []
                first_of = {}
                for pi in range(3):
                    pj = hidx * 3 + pi
                    first_of[len(units)] = pj
                    for bi, (r, j) in enumerate(pblocks(pi)):
                        units.append((pj, pi, bi, r, j, gunit))
                        gunit += 1
                started = [False] * 4
                qk_reads = [("kT", c, gg) for gg in range(4)] + [("qT", c, gg) for gg in range(4)] + [("qz1", c)]

                def stageA(u):
                    pj, pi, bi, r, j, gu = u
                    d, nb = patterns[pi]
                    nq = 2 if j + 1 < nb else 1
                    sb = (4, 5, 7)[gu % 3]
                    pb = Pb[gu % 4]
                    pbk = ("Pb%d" % (gu % 4),)
                    MM(ps[sb][:, 0:nq * 128], kT[:, c, tokslice(d, r, j)], qsrc[h][:, c, tokslice(d, r, j, nq)],
                       True, False, qk_reads, [PS(sb)])
                    MM(ps[sb][:, 0:nq * 128], identB[:], mask2[:, 0:nq * 128], False, True,
                       ["identB", "mask2"], [PS(sb)])
                    ACT(pb[:, 0:nq * 128], ps[sb][:, 0:nq * 128], AF.Exp, (), [PS(sb), pbk])

                def stageB(u):
                    pj, pi, bi, r, j, gu = u
                    d, nb = patterns[pi]
                    nq = 2 if j + 1 < nb else 1
                    pb = Pb[gu % 4]
                    pbk = ("Pb%d" % (gu % 4),)
                    vb = pj % 2
                    for qi in range(nq):
                        jq = j + qi
                        if d == 1:
                            pieces = [(jq // 4, slice((jq % 4) * 128, (jq % 4) * 128 + 128), slice(qi * 128, qi * 128 + 128))]
                        elif d == 4:
                            pieces = [(jq, slice(r, 512, 4), slice(qi * 128, qi * 128 + 128))]
                        else:
                            pieces = [(bb, slice(r, 512, 16), slice(bb * 32, bb * 32 + 32)) for bb in range(4)]
                        for (bank, ocols, pcols) in pieces:
                            MM(ps[bank][0:65, ocols], Vp[vb][:, bi, 0:65], pb[:, pcols], not started[bank], False,
                               [pbk, ("Vpat%d" % vb, bi // 4), ("Vpat%d" % vb, "ones")], [PS(bank)], sgc=True)
                            started[bank] = True

                nu = len(units)
                for i in range(nu + SK):
                    if i < nu:
                        stageA(units[i])
                    if i >= SK:
                        stageB(units[i - SK])
                    f = i - (SK - 1)
                    if f in first_of and first_of[f] + 1 < len(PJ):
                        emit_V(first_of[f] + 1)
                sqs = [(sq65, ("sq65",)), (sqb, "sqb"), (junk[:, 0:512], "junk"), (junk[:, 512:1024], "junk2")]
                mss = [(msf, "msf"), (Cs, "Cs"), (t1, "t1"), (cv, "cv")]
                nbk = [6, 4, 5, 7]
                for bank in range(4):
                    ACT(sqs[bank][0][0:65, :], ps[bank][0:65, :], AF.Square, (), [PS(bank), sqs[bank][1]])
                for bank in range(4):
                    MM(ps[nbk[bank]][0:64, :], wsel[0:65, :], sqs[bank][0][0:65, :], True, True,
                       ["wsel", sqs[bank][1]], [PS(nbk[bank])])
                for bank in range(4):
                    ACT(mss[bank][0][0:64, :], ps[nbk[bank]][0:64, :], AF.Ln, (), [PS(nbk[bank]), mss[bank][1]])
                    ACT(mss[bank][0][0:64, :], mss[bank][0][0:64, :], AF.Exp, [mss[bank][1]], [mss[bank][1]], scale=-0.5)
                for bank in range(4):
                    tk = slice(bank * 512, (bank + 1) * 512)
                    mt, mk = mss[bank]
                    STT(yTa[0:64, head, tk], ps[bank][0:64, :], small[0:64, SM_AG + head:SM_AG + head + 1],
                        mt[0:64, :], ALU.mult, ALU.mult, [mk, "small"], [PS(bank), ("yTa", head, bank)])

            for hd in range(8):
                DMA("pool", woa[0:64, hd, :], woa_d[:, hd, :], (), [("woa", hd)], "woa", max_dma_last_dim=4096)
            for cc in range(4):
                DMA("pool", woc[:, cc, :], woc_d[:, cc, :], (), [("woc", cc)], "woc", max_dma_last_dim=4096)
            R = lambda a, n: rt[:, a:a + n]
            for g in range(4):
                for j in range(4):
                    ti = g * 4 + j
                    tk = slice(ti * 128, (ti + 1) * 128)
                    DMA("sp", xt[j][:], x_d[b, tk, :], (), [("xt%d" % j,)], "xt%d" % j)
                    for half in range(2):
                        bank = (j % 2) * 2 + half
                        cs = slice(half * 512, (half + 1) * 512)
                        for hd in range(8):
                            MM(ps[bank][:, :], yTa[0:64, hd, tk], woa[0:64, hd, cs], hd == 0, False,
                               [("yTa", hd, ti // 4), ("woa", hd)], [PS(bank)])
                        for cc in range(4):
                            MM(ps[bank][:, :], yTc[:, cc, tk], woc[:, cc, cs], False, cc == 3,
                               [("yTc", cc, g), ("woc", cc)], [PS(bank)])
                        TT("dve", tmpf[:], ps[bank][:, :], g1b[:, cs], ALU.mult, [("gb", 0, half)], [PS(bank), ("tmpf",)])
                        TT("dve", xt[j][:, cs], tmpf[:], xt[j][:, cs], ALU.add, [("tmpf",)], [("xt%d" % j,)])
                    DMA("sp", out_d[b, tk, :], xt[j][:], [("xt%d" % j,)], [("x1d", b, ti)], "x1st")
                norm_and_transpose(g, sc2, sh2, lambda k: h2T[:, k, g * 512:(g + 1) * 512], lambda k: ("h2T", k, g),
                                   dst_f32=h2f, tbank=(4, 5))
                for k in range(8):
                    MM(ps[6][0:64, :], wr[:, k, :], h2f[:, k, :], k == 0, k == 7, [("h2f", k), "wr"], [PS(6)])
                ACT(tmpf[0:64, :], ps[6][0:64, :], AF.Copy, (), [PS(6), ("tmpf",)])
                for j in range(4):
                    TR(ps[7][:, j * 64:(j + 1) * 64], tmpf[0:64, j * 128:(j + 1) * 128], identF[0:64, 0:64],
                       [("tmpf",), "identF"], [PS(7)])
                for j in range(4):
                    ti = g * 4 + j
                    en, sg_, bs, bs2, mb, sel = R(0, 64), R(64, 64), R(128, 64), R(192, 64), R(256, 64), R(320, 64)
                    m1, m2, gs, g8, gm, e8 = R(384, 8), R(392, 8), R(400, 8), R(408, 8), R(416, 8), R(424, 8)
                    ds_, rc = R(432, 1), R(433, 1)
                    RK = [("rt",)]
                    ACT(en, ps[7][:, j * 64:(j + 1) * 64], AF.Exp, (), [PS(7)] + RK, scale=-1.0)
                    TS("dve", en, en, 1.0, None, ALU.add, None, RK, RK)
                    P.add("dve", (lambda o, i: (lambda e: e.reciprocal(out=o, in_=i)))(sg_, en), RK, RK)
                    TT("dve", bs, sg_, small[:, SM_RB:SM_RB + 64], ALU.add, RK + ["small"], RK)
                    bs3 = bs.rearrange("p (a c) -> p a c", a=8)
                    bs23 = bs2.rearrange("p (a c) -> p a c", a=8)
                    mb3 = mb.rearrange("p (a c) -> p a c", a=8)
                    P.add("dve", (lambda o, i: (lambda e: e.tensor_reduce(out=o, in_=i, axis=AX.X, op=ALU.max)))(m1, bs3), RK, RK)
                    TT("dve", bs23, bs3, m1.unsqueeze(2).to_broadcast([128, 8, 8]), ALU.is_equal, RK, RK)
                    STT(bs2, bs2, -BIG, bs, ALU.mult, ALU.add, RK, RK)
                    P.add("dve", (lambda o, i: (lambda e: e.tensor_reduce(out=o, in_=i, axis=AX.X, op=ALU.max)))(m2, bs23), RK, RK)
                    TT("dve", gs, m1, m2, ALU.add, RK, RK)
                    P.add("dve", (lambda o, i: (lambda e: e.max(out=o, in_=i)))(g8, gs), RK, RK)
                    TS("dve", gm, gs, g8[:, 3:4], None, ALU.is_ge, None, RK, RK)
                    TS("dve", gm, gm, -1.0, BIG, ALU.add, ALU.mult, RK, RK)
                    TT("dve", mb3, bs3, gm.unsqueeze(2).to_broadcast([128, 8, 8]), ALU.add, RK, RK)
                    P.add("dve", (lambda o, i: (lambda e: e.max(out=o, in_=i)))(e8, mb), RK, RK)
                    TS("dve", sel, mb, e8[:, 7:8], None, ALU.is_ge, None, RK, RK)
                    TT("dve", sel, sel, sg_, ALU.mult, RK, RK)
                    P.add("dve", (lambda o, i: (lambda e: e.tensor_reduce(out=o, in_=i, axis=AX.X, op=ALU.add)))(ds_, sel), RK, RK)
                    P.add("dve", (lambda o, i: (lambda e: e.reciprocal(out=o, in_=i)))(rc, ds_), RK, RK)
                    TS("dve", gates[:, ti, 0:64], sel, rc, 2.5, ALU.mult, ALU.mult, RK, [("gates", ti)])

            def load_expert(e):
                i = e % 2
                DMA("pool", wgb[i][:], wg_d[e], (), [("wgb%d" % i,)], "wg%d" % i, max_dma_last_dim=4096)
                DMA("pool", wub[i][:], wu_d[e], (), [("wub%d" % i,)], "wu%d" % i, max_dma_last_dim=4096)
                DMA("pool", wdb[i][:], wd_d[e], (), [("wdb%d" % i,)], "wd%d" % i, max_dma_last_dim=4096)

            load_expert(0)
            drot = [0]

            def GU(e, g, fc, par):
                i = e % 2
                tok = slice(g * 512, (g + 1) * 512)
                fs = slice(fc * 128, (fc + 1) * 128)
                bA, bB = fc, 2 + fc
                for k in range(8):
                    MM(ps[bA][:, :], wgb[i][:, k, fs], h2T[:, k, tok], k == 0, k == 7,
                       [("wgb%d" % i,), ("h2T", k, g)], [PS(bA)])
                for k in range(8):
                    MM(ps[bB][:, :], wub[i][:, k, fs], h2T[:, k, tok], k == 0, k == 7,
                       [("wub%d" % i,), ("h2T", k, g)], [PS(bB)])
                ACT(sg[fc][:], ps[bA][:, :], AF.Silu, (), [PS(bA), ("sg%d" % fc,)])
                TT("dve", hid[par][fc][:], ps[bB][:, :], sg[fc][:], ALU.mult, [("sg%d" % fc,)],
                   [PS(bB), ("hid%d_%d" % (par, fc),)])

            def DOWN(e, g, par):
                i = e % 2
                for j in range(4):
                    ti = g * 4 + j
                    for half in range(2):
                        bank = 4 + drot[0] % 4
                        drot[0] += 1
                        cs = slice(half * 512, (half + 1) * 512)
                        for fc in range(2):
                            MM(ps[bank][:, :], hid[par][fc][:, j * 128:(j + 1) * 128], wdb[i][:, fc, cs],
                               fc == 0, fc == 1, [("hid%d_%d" % (par, fc),), ("wdb%d" % i,)], [PS(bank)])
                        gk = [("gates", ti)] if e < 64 else ["gates_sh"]
                        if e == 0:
                            TS("dve", acc[:, ti, cs], ps[bank][:, :], gates[:, ti, e:e + 1], None, ALU.mult, None,
                               gk, [PS(bank), ("acc", ti, half)])
                        else:
                            STT(acc[:, ti, cs], ps[bank][:, :], gates[:, ti, e:e + 1], acc[:, ti, cs],
                                ALU.mult, ALU.add, gk, [PS(bank), ("acc", ti, half)])

            def load_x1(ti):
                tk_ = slice(ti * 128, (ti + 1) * 128)
                DMA("sp", xt2[ti % 4][:], out_d[b, tk_, :], [("x1d", b, ti)], [("xt2_%d" % (ti % 4),)],
                    "x1ld%d" % (ti % 4))

            munits = [(e, g) for e in range(n_exp) for g in range(4)]
            for idx, (e, g) in enumerate(munits):
                if idx == len(munits) - 4:
                    for ti0 in range(4):
                        load_x1(ti0)
                GU(e, g, 0, idx % 2)
                if idx > 0:
                    pe_, pg_ = munits[idx - 1]
                    DOWN(pe_, pg_, (idx - 1) % 2)
                if g == 0 and e + 1 < n_exp:
                    load_expert(e + 1)
                GU(e, g, 1, idx % 2)
            pe_, pg_ = munits[-1]
            DOWN(pe_, pg_, (len(munits) - 1) % 2)

            for ti in range(16):
                tk = slice(ti * 128, (ti + 1) * 128)
                xb_ = xt2[ti % 4]
                xk = ("xt2_%d" % (ti % 4),)
                for half in range(2):
                    cs = slice(half * 512, (half + 1) * 512)
                    TT("dve", acc[:, ti, cs], acc[:, ti, cs], g2b[:, cs], ALU.mult, [("gb", 1, half)], [("acc", ti, half)])
                    TT("pool" if half else "dve", acc[:, ti, cs], acc[:, ti, cs], xb_[:, cs], ALU.add, [xk],
                       [("acc", ti, half)])
                DMA("sp", out_d[b, tk, :], acc[:, ti, :], [("acc", ti, 0), ("acc", ti, 1)], [("x1d", b, ti)],
                    "outst", is_output=True)
                if ti + 4 < 16:
                    load_x1(ti + 4)

        P.emit(ctx)
    return nc


_PROG = None


def _layouts(inp):
    f = lambda a: np.ascontiguousarray(np.asarray(a, dtype=np.float32))
    L = {}
    L["wr"] = f(inp["w_router"][0].reshape(8, 128, 64).transpose(1, 0, 2))
    L["wada"] = f(inp["w_ada"][0].reshape(8, 128, 6144))
    L["bada"] = f(inp["b_ada"][0].reshape(1, 6144))
    L["win"] = f(inp["w_in"][0].reshape(8, 128, 3072).transpose(1, 0, 2))
    wo = inp["w_out"][0]
    L["woa"] = f(wo[:512].reshape(8, 64, 1024).transpose(1, 0, 2))
    L["woc"] = f(wo[512:].reshape(4, 128, 1024).transpose(1, 0, 2))
    wg = np.concatenate([inp["w_e_gate"][0], inp["w_s_gate"][0][None]], 0)
    wu = np.concatenate([inp["w_e_up"][0], inp["w_s_up"][0][None]], 0)
    wd = np.concatenate([inp["w_e_down"][0], inp["w_s_down"][0][None]], 0)
    L["wg"] = f(wg.reshape(NE, 8, 128, 256).transpose(0, 2, 1, 3))
    L["wu"] = f(wu.reshape(NE, 8, 128, 256).transpose(0, 2, 1, 3))
    L["wd"] = f(wd.reshape(NE, 2, 128, 1024).transpose(0, 2, 1, 3))
    sm = np.zeros((128, SM_N), np.float32)
    sm[:, SM_QG] = np.tile(inp["q_norm_g"][0], 2)
    sm[:, SM_KG] = np.tile(inp["k_norm_g"][0], 2)
    sm[:, SM_CW:SM_CW + 12] = inp["conv_w"][0].reshape(3, 4, 128).transpose(2, 1, 0).reshape(128, 12)
    sm[:64, SM_AG:SM_AG + 8] = inp["attn_out_g"][0].reshape(8, 64).T
    sm[:, SM_CG:SM_CG + 4] = inp["conv_out_g"][0].reshape(4, 128).T
    sm[:, SM_RB:SM_RB + 64] = np.broadcast_to(inp["router_bias"][0][None, :], (128, 64))
    sm[:, SM_BA:SM_BA + 48] = inp["b_ada"][0].reshape(48, 128).T
    L["small"] = sm
    return L


def kernel(**inputs):
    global _PROG
    inp = {k: np.asarray(v) for k, v in inputs.items()}
    L = _layouts(inp)
    x = np.ascontiguousarray(inp["x"], dtype=np.float32)
    c = np.asarray(inp["c"], dtype=np.float32)
    if _PROG is None:
        _PROG = build_program()
    in_maps = []
    for core in range(NCORES):
        m = dict(L)
        sm = L["small"].copy()
        cc = c[2 * core:2 * core + 2]
        sm[:, SM_CT:SM_CT + 16] = cc.reshape(2, 8, 128).transpose(2, 1, 0).reshape(128, 16)
        m["small"] = sm
        m["x"] = x[2 * core:2 * core + 2]
        in_maps.append(m)
    res = run_bass_kernel_spmd(_PROG, in_maps, core_ids=list(range(NCORES)))
    out = np.concatenate([r["out"] for r in res.results], axis=0)
    return out.astype(np.float32, copy=False)
```
